# Optimizing a Trainium2 kernel written in Bass

```python
import jax, jax.numpy as jnp
from jax import lax
import numpy as np

D_MODEL = 1024
BATCH = 8
SEQ = 4096
DEPTH = 2

N_A = DEPTH // 2
N_B = DEPTH - N_A
N_META = 16
POOL_WINDOWS = (2, 4, 8, 16)
N_POOL_GROUPS = len(POOL_WINDOWS)
POOL_GROUP_DIM = D_MODEL // N_POOL_GROUPS
MAX_WINDOW = max(POOL_WINDOWS)
N_HEADS = 8
QK_NOPE_DIM = 128
QK_ROPE_DIM = 64
V_HEAD_DIM = 128
Q_LORA_RANK = 384
KV_LORA_RANK = 256
ROPE_THETA = 10000.0
ATTN_SCALE = (QK_NOPE_DIM + QK_ROPE_DIM) ** -0.5
Q_BLOCK = 128
N_EXPERT_GROUPS = 4
EXPERTS_PER_GROUP = 8
N_EXPERTS = N_EXPERT_GROUPS * EXPERTS_PER_GROUP
TOP_K_IN_GROUP = 2
D_EXPERT = 256
MOE_GROUP_ROWS = 256
RMS_EPS = 1e-6
NEG_INF = -1e30

kernel_name = "yoco_pool_mla_hier_moe"


def rmsnorm(x, g):
    xf = x.astype(jnp.float32)
    y = xf * lax.rsqrt(jnp.mean(xf * xf, axis=-1, keepdims=True) + RMS_EPS)
    return (y * g.astype(jnp.float32)).astype(x.dtype)


def rope_tables(length):
    pos = jnp.arange(length, dtype=jnp.float32)
    inv_freq = ROPE_THETA ** (-jnp.arange(0, QK_ROPE_DIM, 2, dtype=jnp.float32) / QK_ROPE_DIM)
    ang = pos[:, None] * inv_freq[None, :]
    return jnp.cos(ang), jnp.sin(ang)


def apply_rope(x, cos, sin):
    xf = x.astype(jnp.float32)
    half = xf.shape[-1] // 2
    x1, x2 = xf[..., :half], xf[..., half:]
    return jnp.concatenate([x1 * cos - x2 * sin, x1 * sin + x2 * cos], axis=-1).astype(x.dtype)


def pool_mixer(h, w_grp, scale):
    L_ = h.shape[1]
    hf = h.astype(jnp.float32)
    cs = jnp.pad(jnp.cumsum(hf, axis=1), ((0, 0), (MAX_WINDOW, 0), (0, 0)))
    t = jnp.arange(L_)
    outs = []
    for g, w in enumerate(POOL_WINDOWS):
        sl = slice(g * POOL_GROUP_DIM, (g + 1) * POOL_GROUP_DIM)
        win_sum = cs[:, MAX_WINDOW:, sl] - cs[:, MAX_WINDOW - w:MAX_WINDOW - w + L_, sl]
        cnt = jnp.minimum(t + 1, w).astype(jnp.float32)[None, :, None]
        pooled = (win_sum / cnt - hf[:, :, sl]).astype(h.dtype)
        outs.append(jnp.einsum('blc,cd->bld', pooled, w_grp[g]))
    return jnp.concatenate(outs, axis=-1) * scale


def shared_latent_kv(h, kv_norm, w_dkv, kv_lat_norm, w_uk, w_uv, cos, sin):
    c = jnp.einsum('bld,dr->blr', rmsnorm(h, kv_norm), w_dkv)
    c_kv = rmsnorm(c[..., :KV_LORA_RANK], kv_lat_norm)
    k_rope = apply_rope(c[..., KV_LORA_RANK:], cos, sin)
    k_nope = jnp.einsum('blr,rhd->blhd', c_kv, w_uk)
    v = jnp.einsum('blr,rhd->blhd', c_kv, w_uv)
    return k_nope, k_rope, v


def causal_block_attention(q_nope, q_rope, k_nope, k_rope, v):
    B_, L_, H_, _ = q_nope.shape
    nb = -(-L_ // Q_BLOCK)
    Lp = nb * Q_BLOCK
    pad = Lp - L_
    padf = lambda a: jnp.pad(a, [(0, 0), (0, pad)] + [(0, 0)] * (a.ndim - 2))
    qn, qr, kn, kr, vv = padf(q_nope), padf(q_rope), padf(k_nope), padf(k_rope), padf(v)
    to_blocks = lambda a: jnp.moveaxis(a.reshape((B_, nb, Q_BLOCK) + a.shape[2:]), 1, 0)
    kpos = jnp.arange(Lp)

    def one_block(args):
        qn_b, qr_b, b_idx = args
        s = (jnp.einsum('bqhd,bkhd->bhqk', qn_b, kn, preferred_element_type=jnp.float32)
             + jnp.einsum('bqhr,bkr->bhqk', qr_b, kr, preferred_element_type=jnp.float32)) * ATTN_SCALE
        qpos = b_idx * Q_BLOCK + jnp.arange(Q_BLOCK)
        s = jnp.where(kpos[None, :] <= qpos[:, None], s, NEG_INF)
        p = jax.nn.softmax(s, axis=-1)
        return jnp.einsum('bhqk,bkhd->bqhd', p.astype(vv.dtype), vv)

    o = lax.map(one_block, (to_blocks(qn), to_blocks(qr), jnp.arange(nb)))
    return jnp.moveaxis(o, 0, 1).reshape(B_, Lp, H_, V_HEAD_DIM)[:, :L_]


def mla_mixer(hn, w_dq, q_norm, w_uq, w_o, k_nope, k_rope, v, cos, sin):
    B_, L_, _ = hn.shape
    cq = rmsnorm(jnp.einsum('bld,dr->blr', hn, w_dq), q_norm)
    q = jnp.einsum('blr,rhd->blhd', cq, w_uq)
    q_nope = q[..., :QK_NOPE_DIM]
    q_rope = apply_rope(q[..., QK_NOPE_DIM:], cos[:, None, :], sin[:, None, :])
    o = causal_block_attention(q_nope, q_rope, k_nope, k_rope, v)
    return jnp.einsum('blf,fd->bld', o.reshape(B_, L_, N_HEADS * V_HEAD_DIM), w_o)


def hierarchical_moe(h, wr_g, br_g, wr_e, br_e, w_gate, w_up, w_down):
    B_, L_, D_ = h.shape
    xt = h.reshape(-1, D_)
    n_tok = xt.shape[0]
    xf = xt.astype(jnp.float32)
    p_g = jax.nn.softmax(xf @ wr_g.astype(jnp.float32) + br_g.astype(jnp.float32), axis=-1)
    g_sel = jnp.argmax(p_g, axis=-1)
    w_g = jnp.max(p_g, axis=-1)
    logit_e = (xf @ wr_e.astype(jnp.float32) + br_e.astype(jnp.float32)).reshape(n_tok, N_EXPERT_GROUPS, EXPERTS_PER_GROUP)
    sel = jnp.take_along_axis(logit_e, g_sel[:, None, None], axis=1)[:, 0]
    top_v, top_i = lax.top_k(sel, TOP_K_IN_GROUP)
    gate = w_g[:, None] * jax.nn.softmax(top_v, axis=-1)
    eid = (g_sel[:, None] * EXPERTS_PER_GROUP + top_i).reshape(-1).astype(jnp.int32)
    tid = jnp.repeat(jnp.arange(n_tok, dtype=jnp.int32), TOP_K_IN_GROUP)
    wt = gate.reshape(-1)
    n_assign = n_tok * TOP_K_IN_GROUP
    order = jnp.argsort(eid)
    e_sorted = eid[order]
    counts = jnp.bincount(eid, length=N_EXPERTS)
    starts = jnp.cumsum(counts) - counts
    padded = (counts + MOE_GROUP_ROWS - 1) // MOE_GROUP_ROWS * MOE_GROUP_ROWS
    pends = jnp.cumsum(padded)
    pstarts = pends - padded
    dest = pstarts[e_sorted] + (jnp.arange(n_assign) - starts[e_sorted])
    n_blocks = -(-(n_assign + N_EXPERTS * (MOE_GROUP_ROWS - 1)) // MOE_GROUP_ROWS)
    n_rows = n_blocks * MOE_GROUP_ROWS
    row_tok = jnp.full((n_rows,), n_tok, jnp.int32).at[dest].set(tid[order])
    row_w = jnp.zeros((n_rows,), h.dtype).at[dest].set(wt[order].astype(h.dtype))
    block_e = jnp.minimum(jnp.searchsorted(pends, jnp.arange(n_blocks) * MOE_GROUP_ROWS, side='right'), N_EXPERTS - 1)
    x_rows = jnp.concatenate([xt, jnp.zeros((1, D_), xt.dtype)], axis=0)[row_tok]
    x_rows = x_rows.reshape(n_blocks, MOE_GROUP_ROWS, D_)

    def expert_block(args):
        e, xb = args
        return (jax.nn.silu(xb @ w_gate[e]) * (xb @ w_up[e])) @ w_down[e]

    y = lax.map(expert_block, (block_e, x_rows)).reshape(n_rows, D_)
    out = jnp.zeros((n_tok + 1, D_), h.dtype).at[row_tok].add(y * row_w[:, None])[:n_tok]
    return out.reshape(B_, L_, D_)


def setup_inputs(seed: int = 0) -> dict:
    key = jax.random.key(seed)
    ks = jax.random.split(key, 32)
    f32 = jnp.float32
    nrm = lambda k, shape, fan: jax.random.normal(k, shape, f32) * (fan ** -0.5)
    gain = lambda k, shape: 1.0 + 0.05 * jax.random.normal(k, shape, f32)
    return {
        "x": jax.random.normal(ks[0], (BATCH, SEQ, D_MODEL), f32),
        "meta_tokens": jax.random.normal(ks[1], (N_META, D_MODEL), f32),
        "a_norm": gain(ks[2], (N_A, D_MODEL)),
        "a_w": nrm(ks[3], (N_A, N_POOL_GROUPS, POOL_GROUP_DIM, POOL_GROUP_DIM), POOL_GROUP_DIM),
        "a_scale": gain(ks[4], (N_A, D_MODEL)),
        "b_norm": gain(ks[5], (N_B, D_MODEL)),
        "b_w_dq": nrm(ks[6], (N_B, D_MODEL, Q_LORA_RANK), D_MODEL),
        "b_q_norm": gain(ks[7], (N_B, Q_LORA_RANK)),
        "b_w_uq": nrm(ks[8], (N_B, Q_LORA_RANK, N_HEADS, QK_NOPE_DIM + QK_ROPE_DIM), Q_LORA_RANK),
        "b_w_o": nrm(ks[9], (N_B, N_HEADS * V_HEAD_DIM, D_MODEL), N_HEADS * V_HEAD_DIM),
        "kv_norm": gain(ks[10], (D_MODEL,)),
        "w_dkv": nrm(ks[11], (D_MODEL, KV_LORA_RANK + QK_ROPE_DIM), D_MODEL),
        "kv_lat_norm": gain(ks[12], (KV_LORA_RANK,)),
        "w_uk": nrm(ks[13], (KV_LORA_RANK, N_HEADS, QK_NOPE_DIM), KV_LORA_RANK),
        "w_uv": nrm(ks[14], (KV_LORA_RANK, N_HEADS, V_HEAD_DIM), KV_LORA_RANK),
        "ffn_norm": gain(ks[15], (DEPTH, D_MODEL)),
        "router_g": nrm(ks[16], (DEPTH, D_MODEL, N_EXPERT_GROUPS), D_MODEL),
        "router_g_bias": 0.01 * jax.random.normal(ks[17], (DEPTH, N_EXPERT_GROUPS), f32),
        "router_e": nrm(ks[18], (DEPTH, D_MODEL, N_EXPERTS), D_MODEL),
        "router_e_bias": 0.01 * jax.random.normal(ks[19], (DEPTH, N_EXPERTS), f32),
        "w_gate": nrm(ks[20], (DEPTH, N_EXPERTS, D_MODEL, D_EXPERT), D_MODEL),
        "w_up": nrm(ks[21], (DEPTH, N_EXPERTS, D_MODEL, D_EXPERT), D_MODEL),
        "w_down": nrm(ks[22], (DEPTH, N_EXPERTS, D_EXPERT, D_MODEL), D_EXPERT),
        "final_norm": gain(ks[23], (D_MODEL,)),
    }


def reference(x, meta_tokens, a_norm, a_w, a_scale, b_norm, b_w_dq, b_q_norm, b_w_uq, b_w_o,
              kv_norm, w_dkv, kv_lat_norm, w_uk, w_uv, ffn_norm, router_g, router_g_bias,
              router_e, router_e_bias, w_gate, w_up, w_down, final_norm):
    B_, S_, D_ = x.shape
    h = jnp.concatenate([jnp.broadcast_to(meta_tokens[None].astype(x.dtype), (B_, N_META, D_)), x], axis=1)
    L_ = S_ + N_META
    cos, sin = rope_tables(L_)
    k_nope = k_rope = v = None
    for layer in range(DEPTH):
        if layer < N_A:
            i = layer
            h = h + pool_mixer(rmsnorm(h, a_norm[i]), a_w[i], a_scale[i])
        else:
            i = layer - N_A
            h = h + mla_mixer(rmsnorm(h, b_norm[i]), b_w_dq[i], b_q_norm[i], b_w_uq[i], b_w_o[i],
                              k_nope, k_rope, v, cos, sin)
        h = h + hierarchical_moe(rmsnorm(h, ffn_norm[layer]), router_g[layer], router_g_bias[layer],
                                 router_e[layer], router_e_bias[layer], w_gate[layer], w_up[layer], w_down[layer])
        if layer == N_A - 1:
            k_nope, k_rope, v = shared_latent_kv(h, kv_norm, w_dkv, kv_lat_norm, w_uk, w_uv, cos, sin)
    return rmsnorm(h, final_norm)[:, N_META:]
```

```python
import numpy as np
import concourse.bass as bass
import concourse.mybir as mybir
from concourse.bass_utils import run_bass_kernel_spmd
from concourse.alu_op_type import AluOpType as ALU

F32 = mybir.dt.float32
BF16 = mybir.dt.bfloat16
I32 = mybir.dt.int32
AF = mybir.ActivationFunctionType
AX = mybir.AxisListType

D = 1024
NT = 33
NE = 32
CAP = 512
NSLOT = NE * CAP
ST = CAP // 128
XW = 514
NYK = NT * 128 * 2
EPS = 1e-6
ATTN_SCALE = 192 ** -0.5
OOB = 0x3FFFFFFF
QC = 4


class Buf:
    __slots__ = ("w", "r")

    def __init__(self):
        self.w = {}
        self.r = {}


class KB:
    def __init__(self, nc, n_dma_sems=14):
        self.nc = nc
        self.eng = {"pe": nc.tensor, "act": nc.scalar, "dve": nc.vector, "pool": nc.gpsimd, "sp": nc.sync}
        self.sem = {}
        self.cnt = {}
        for k in ("pe", "act", "dve", "pool"):
            self.sem[k] = nc.alloc_semaphore("s_" + k)
            self.cnt[k] = 0
        self.dpool, self.dcnt, self.dnext = {}, {}, {}
        for q in ("sp", "pool"):
            self.dpool[q] = [nc.alloc_semaphore(f"d_{q}{i}") for i in range(n_dma_sems)]
            self.dcnt[q] = [0] * n_dma_sems
            self.dnext[q] = 0
        self.seen = {k: {} for k in self.eng}
        self.n_wait = 0
        self.n_inst = 0

    def _wait(self, e, tok):
        sem, val, owner = tok
        key = id(sem)
        if self.seen[e].get(key, 0) >= val:
            return
        if owner == e and e == "pe":
            return
        self.eng[e].wait_ge(sem, val)
        self.seen[e][key] = val
        self.n_wait += 1

    def deps(self, e, reads, writes, shared):
        for b in reads:
            for t in b.w.values():
                self._wait(e, t)
        for b in writes:
            for t in b.w.values():
                self._wait(e, t)
            for t in b.r.values():
                self._wait(e, t)
        for b in shared:
            for t in b.r.values():
                self._wait(e, t)

    def _commit(self, tok, reads, writes, shared):
        k = id(tok[0])
        for b in reads:
            b.r[k] = tok
        for b in writes:
            b.w = {k: tok}
            b.r = {}
        for b in shared:
            b.w[k] = tok

    def op(self, e, fn, reads=(), writes=(), inc=True):
        self.deps(e, reads, writes, ())
        ins = fn()
        self.n_inst += 1
        if inc:
            self.cnt[e] += 1
            ins.then_inc(self.sem[e], 1)
            tok = (self.sem[e], self.cnt[e], e)
        else:
            tok = (self.sem[e], self.cnt[e] + 1, e)
        self._commit(tok, reads, writes, ())
        return tok

    def dma(self, q, fn, reads=(), writes=(), shared=()):
        i = self.dnext[q]
        self.dnext[q] = (i + 1) % len(self.dpool[q])
        sem = self.dpool[q][i]
        if self.dcnt[q][i] > 0:
            self._wait(q, (sem, self.dcnt[q][i], "dma"))
        self.deps(q, reads, writes, shared)
        ins = fn(self.eng[q])
        self.n_inst += 1
        self.dcnt[q][i] += 16
        ins.then_inc(sem, 16)
        tok = (sem, self.dcnt[q][i], "dma")
        self._commit(tok, reads, writes, shared)
        return tok

    def finish(self, bufs):
        for b in bufs:
            for t in list(b.w.values()) + list(b.r.values()):
                self._wait("sp", t)


class Tl:
    def __init__(self, nc, name, shape, dtype, psum=False):
        name = "t_" + name
        self.t = nc.alloc_psum_tensor(name, shape, dtype) if psum else nc.alloc_sbuf_tensor(name, shape, dtype)
        self.b = Buf()

    def __getitem__(self, k):
        return self.t[k]


def _consts():
    c = {}
    pool = np.zeros((3, 4, 128, 128), np.float32)
    i = np.arange(128)[:, None]
    o = np.arange(128)[None, :]
    for g, w in enumerate((2, 4, 8, 16)):
        pool[0, g] = ((i > o - w) & (i <= o)) / w - (i == o)
        pool[1, g] = (i - 128 > o - w) / w
        pi, po = i - 112, o - 112
        inw = (pi >= 0) & (po >= 0) & (pi > po - w) & (pi <= po)
        cnt = np.minimum(po + 1, w).astype(np.float32)
        cnt = np.where(po >= 0, cnt, 1.0)
        pool[2, g] = inw / cnt - ((i == o) & (po >= 0))
    c["c_pool"] = pool
    c["c_lt"] = (i < o).astype(np.float32)
    e = np.arange(NE, dtype=np.float32)[None, :]
    c["c_slot"] = np.concatenate([np.broadcast_to(e * CAP, (128, NE)), np.broadcast_to((e + 1) * CAP, (128, NE))], 1).astype(np.float32)
    p = np.arange(128)
    ret = np.zeros((128, NT, 2), np.int32)
    for t in range(NT):
        for k in range(2):
            ret[:, t, k] = (t * 128 + p) * 2 + k
    c["c_ret"] = ret
    v0 = np.zeros((128, 2), np.float32)
    v0[:, 0] = (p >= 112)
    v0[:, 1] = (p < 112) * 1e7
    c["c_valid0"] = v0
    gid = np.arange(NT * 128)
    pos = np.where(gid < 128, gid - 112, 16 + gid - 128).astype(np.float32)
    pos = np.maximum(pos, 0.0).astype(np.float32)
    inv = (np.float32(10000.0) ** (-np.arange(0, 64, 2, dtype=np.float32) / np.float32(64))).astype(np.float32)
    ang = (pos[None, :] * inv[:, None]).astype(np.float32)
    cs, sn = np.cos(ang).astype(np.float32), np.sin(ang).astype(np.float32)
    rope = np.zeros((2, 64, NT * 128), np.float32)
    rope[0, :32], rope[0, 32:] = cs, cs
    rope[1, :32], rope[1, 32:] = -sn, sn
    c["c_rope"] = rope
    c["c_mask"] = np.where(i > o, -30000.0, 0.0).astype(np.float32)
    xi = np.zeros((128, NSLOT // 128, 2), np.int32)
    xi[:, :, 0] = OOB
    c["c_xinit"] = xi
    return c


WEIGHT_SPECS = {
    "x": ([4096, D], F32), "meta_tokens": ([16, D], F32),
    "a_w": ([4, 256, 256], F32), "gains": ([128, 7, D], F32),
    "qn_gain": ([128, 384], F32), "kvl_gain": ([128, 256], F32),
    "b_w_dq": ([D, 384], F32), "b_w_uq": ([384, 8, 192], F32), "b_w_o": ([D, D], F32),
    "w_dkv": ([D, 320], F32), "w_uk": ([256, D], F32), "w_uv": ([256, D], F32),
    "router_w": ([2, D, 36], F32), "router_b": ([128, 2, 36], F32),
    "w_gate": ([2, NE, D, 256], F32), "w_up": ([2, NE, D, 256], F32), "w_down": ([2, NE, 256, D], F32),
    "c_pool": ([3, 4, 128, 128], F32), "c_lt": ([128, 128], F32), "c_slot": ([128, 64], F32),
    "c_ret": ([128, NT, 2], I32), "c_valid0": ([128, 2], F32), "c_rope": ([2, 64, NT * 128], F32),
    "c_mask": ([128, 128], F32), "c_xinit": ([128, NSLOT // 128, 2], I32),
}
G_ANORM, G_ASCALE, G_FFN0, G_FFN1, G_KV, G_B, G_FINAL = range(7)


def build_program(stop_after="all", dbg={}):
    nc = bass.Bass("TRN2", target_bir_lowering=False)
    kb = KB(nc)
    I = {k: nc.dram_tensor(k, s, d, kind="ExternalInput") for k, (s, d) in WEIGHT_SPECS.items()}
    out_d = nc.dram_tensor("out", [4096, D], F32, kind="ExternalOutput")
    b_out = Buf()
    h_d = nc.dram_tensor("h_scr", [NT * 128, D], F32)
    b_h = [Buf() for _ in range(NT)]
    xs_d = [nc.dram_tensor(f"xs_scr{l}", [NSLOT, XW], I32) for l in range(2)]
    b_xs = [Buf(), Buf()]
    b_xsi = [Buf(), Buf()]
    yk_d = [nc.dram_tensor(f"yk_scr{l}", [NYK, D], F32) for l in range(2)]
    b_yk = [Buf(), Buf()]
    kt_d = nc.dram_tensor("kt_scr", [8, 128, NT, 128], BF16)
    kr_d = nc.dram_tensor("kr_scr", [64, NT, 128], BF16)
    vt_d = nc.dram_tensor("vt_scr", [8, 128, NT, 130], BF16)
    b_kv = [Buf() for _ in range(NT)]
    dbg_d = {}
    reg_xs = nc.gpsimd.to_reg(NSLOT - 1)
    reg_yk = nc.gpsimd.to_reg(NYK - 1)

    def T(name, shape, dtype, psum=False):
        return Tl(nc, name, shape, dtype, psum)

    bl = lambda xs: [x.b if isinstance(x, Tl) else x for x in xs]

    def V(fn, r=(), w=(), **k):
        return kb.op("dve", fn, bl(r), bl(w), **k)

    def A(fn, r=(), w=(), **k):
        return kb.op("act", fn, bl(r), bl(w), **k)

    def G(fn, r=(), w=(), **k):
        return kb.op("pool", fn, bl(r), bl(w), **k)

    def PE(fn, r=(), w=(), **k):
        return kb.op("pe", fn, bl(r), bl(w), **k)

    def DMA(q, fn, r=(), w=(), s=()):
        return kb.dma(q, fn, bl(r), bl(w), bl(s))

    def ld(q, dst, dst_ap, src_ap, r=()):
        return DMA(q, lambda e: e.dma_start(out=dst_ap, in_=src_ap), r=r, w=[dst])

    ident = T("ident", [128, 128], BF16)
    identf = T("identf", [128, 128], F32)
    onesb = T("onesb", [128, 128], BF16)
    ltb = T("ltb", [128, 128], BF16)
    slotc = T("slotc", [128, 64], F32)
    retc = T("retc", [128, NT, 2], I32)
    valid0 = T("valid0", [128, 2], F32)
    rbias = T("rbias", [128, 2, 36], F32)
    maskb = T("maskb", [128, 128], BF16)
    base = T("base", [128, NE], F32)
    wr = [T(f"wr{l}", [128, 8, 36], F32) for l in range(2)]
    PS = [T(f"ps{i}", [128, 512], F32, psum=True) for i in range(8)]

    G(lambda: nc.gpsimd.iota(identf[:, :], pattern=[[1, 128]], base=0, channel_multiplier=-1,
                             allow_small_or_imprecise_dtypes=True), w=[identf])
    V(lambda: nc.vector.tensor_single_scalar(out=ident[:, :], in_=identf[:, :], scalar=0.0, op=ALU.is_equal), r=[identf], w=[ident])
    V(lambda: nc.vector.tensor_single_scalar(out=identf[:, :], in_=identf[:, :], scalar=0.0, op=ALU.is_equal), r=[identf], w=[identf])
    V(lambda: nc.vector.memset(onesb[:, :], 1.0), w=[onesb])
    ld("pool", ltb, ltb[:, :], I["c_lt"].ap())
    ld("pool", maskb, maskb[:, :], I["c_mask"].ap())
    ld("sp", slotc, slotc[:, :], I["c_slot"].ap())
    ld("sp", retc, retc[:, :, :], I["c_ret"].ap())
    ld("sp", valid0, valid0[:, :], I["c_valid0"].ap())
    ld("sp", rbias, rbias[:, :, :], I["router_b"].ap())
    for l in range(2):
        ld("sp", wr[l], wr[l][:, :, :], I["router_w"].ap()[l].rearrange("(c p) n -> p c n", p=128))

    ARENA_W = (nc.sbuf_bytes_remaining - 1024) // 4
    arena = nc.alloc_sbuf_tensor("arena", [128, ARENA_W], I32)
    ar = {"off": 0, "n": 0}
    DSZ = {F32: 4, I32: 4, BF16: 2}

    def AT(name, shape, dtype):
        n = int(np.prod(shape[1:]))
        words = (n * DSZ[dtype] + 3) // 4
        words = (words + 7) // 8 * 8
        off = ar["off"]
        ar["off"] += words
        assert ar["off"] <= ARENA_W, f"arena overflow at {name}: {ar['off']} > {ARENA_W}"
        v = arena[0:shape[0], off:off + words]
        if dtype != I32:
            v = v.bitcast(dtype)
        v = v[:, 0:n]
        if len(shape) == 3:
            v = v.rearrange("p (a b) -> p a b", a=shape[1])
        elif len(shape) == 4:
            v = v.rearrange("p (a b c) -> p a b c", a=shape[1], b=shape[2])
        t = Tl.__new__(Tl)
        t.t = v
        t.b = Buf()
        return t

    def phase(name):
        toks = [(kb.sem[e], kb.cnt[e], e) for e in ("pe", "act", "dve", "pool") if kb.cnt[e] > 0]
        for q in ("sp", "pool"):
            toks += [(kb.dpool[q][i], kb.dcnt[q][i], "dma") for i in range(len(kb.dpool[q])) if kb.dcnt[q][i] > 0]
        for e in ("pe", "act", "dve", "pool", "sp"):
            for tk in toks:
                if tk[2] == e and e != "pe":
                    kb.eng[e].wait_ge(tk[0], tk[1])
                    kb.seen[e][id(tk[0])] = tk[1]
                elif tk[2] != e:
                    kb._wait(e, tk)
        ar["off"] = 0

    class Rot:
        def __init__(self, name, shape, dtype, n, alloc=None):
            alloc = alloc or AT
            self.l = [alloc(f"{name}{i}", shape, dtype) for i in range(n)]
            self.i = 0

        def get(self):
            t = self.l[self.i % len(self.l)]
            self.i += 1
            return t

    cur = {}

    def rstd_of(src_tl, src_ap, n):
        s = cur["st"].get()
        junk = cur["junk"]
        V(lambda: nc.vector.scalar_tensor_tensor(out=junk[:, 0:n], in0=src_ap, scalar=1.0, in1=src_ap,
                                                 op0=ALU.mult, op1=ALU.mult, accum_out=s[:, 0:1]), r=[src_tl], w=[junk, s])
        V(lambda: nc.vector.tensor_scalar(out=s[:, 1:2], in0=s[:, 0:1], scalar1=1.0 / n, scalar2=EPS, op0=ALU.mult, op1=ALU.add), r=[s], w=[s])
        A(lambda: nc.scalar.activation(out=s[:, 2:3], in_=s[:, 1:2], func=AF.Ln), r=[s], w=[s])
        A(lambda: nc.scalar.activation(out=s[:, 3:4], in_=s[:, 2:3], func=AF.Exp, scale=-0.5), r=[s], w=[s])
        return s

    def common_bufs():
        cur["st"] = Rot("st", [128, 8], F32, 8)
        cur["junk"] = AT("junk", [128, D], BF16)

    def dbg_out(name, tl, ap, shape, dtype=F32):
        d = nc.dram_tensor("dbg_" + name, shape, dtype, kind="ExternalOutput")
        b = Buf()
        DMA("sp", lambda e: e.dma_start(out=d.ap(), in_=ap), r=[tl], w=[b])
        dbg_d[name] = b

    def dbg_dram(name, src, shape, dtype, deps):
        d = nc.dram_tensor("dbg_" + name, shape, dtype, kind="ExternalOutput")
        b = Buf()
        DMA("sp", lambda e: e.dma_start(out=d.ap(), in_=src), r=deps, w=[b])
        dbg_d[name] = b

    def finish(extra):
        kb.finish(list(extra) + list(dbg_d.values()))
        print("instructions", kb.n_inst, "waits", kb.n_wait, "arena words", ARENA_W)
        return nc

    def idle(n):
        for _ in range(n):
            yield "idle"

    def pipeline(make_gens, depth, skew):
        it = iter(make_gens)
        active = []
        done = False
        while True:
            while not done and len(active) < depth and (not active or active[-1][1] >= skew):
                mk = next(it, None)
                if mk is None:
                    done = True
                    break
                active.append([mk(), 0])
            if not active:
                break
            for a in list(active):
                try:
                    next(a[0])
                    a[1] += 1
                except StopIteration:
                    active.remove(a)

    def route_bufs(gain_tl, gain_ap, n=2):
        return dict(xnf=Rot("xnf", [128, D], F32, n), xnT=Rot("xnT", [128, 8, 128], F32, n),
                    xs=[AT(f"xs{i}", [128, 2, XW], I32) for i in range(n)], rt=Rot("rt", [128, 160], F32, 2),
                    desti=Rot("desti", [128, 2], I32, 4), Ab=Rot("Ab", [128, NE], BF16, n), gain_tl=gain_tl, gain_ap=gain_ap)

    def route_and_scatter(l, t, h_tl, h_ap, psT0, psT1, psS, rb, bgslack=0):
        xnf, xnT, Ab = rb["xnf"].get(), rb["xnT"].get(), rb["Ab"].get()
        s = rstd_of(h_tl, h_ap, D)
        V(lambda: nc.vector.scalar_tensor_tensor(out=xnf[:, :], in0=h_ap, scalar=s[:, 3:4], in1=rb["gain_ap"],
                                                 op0=ALU.mult, op1=ALU.mult), r=[h_tl, s, rb["gain_tl"]], w=[xnf])
        x2 = rb["xs"][t % len(rb["xs"])]
        xb = x2[:, :, :].bitcast(BF16)
        A(lambda: nc.scalar.activation(out=xb[:, 0, 0:D], in_=xnf[:, :], func=AF.Copy), r=[xnf], w=[x2])
        A(lambda: nc.scalar.activation(out=xb[:, 1, 0:D], in_=xnf[:, :], func=AF.Copy), r=[xnf], w=[x2])
        yield
        yield from idle(3 * bgslack)
        for half, ps in ((0, psT0), (1, psT1)):
            for c in range(4 * half, 4 * half + 4):
                PE(lambda c=c, ps=ps: nc.tensor.transpose(out=ps[:, (c % 4) * 128:(c % 4 + 1) * 128], in_=xnf[:, c * 128:(c + 1) * 128],
                                                          identity=identf[:, :]), r=[xnf, identf], w=[ps], inc=(c % 4 == 3))
            if half == 0:
                A(lambda: nc.scalar.activation(out=xnT[:, 0:4, :], in_=psT0[:, :].rearrange("p (c k) -> p c k", c=4), func=AF.Copy), r=[psT0], w=[xnT])
            else:
                V(lambda: nc.vector.tensor_copy(out=xnT[:, 4:8, :], in_=psT1[:, :].rearrange("p (c k) -> p c k", c=4)), r=[psT1], w=[xnT])
        yield
        yield from idle(bgslack)
        for c in range(8):
            PE(lambda c=c: nc.tensor.matmul(psS[:, 0:36], lhsT=xnT[:, c, :], rhs=wr[l][:, c, :], start=(c == 0), stop=(c == 7)),
               r=[xnT, wr[l]], w=[psS], inc=(c == 7))
        r = rb["rt"].get()
        yield
        V(lambda: nc.vector.tensor_tensor(out=r[:, 0:36], in0=psS[:, 0:36], in1=rbias[:, l, :], op=ALU.add), r=[psS, rbias], w=[r])
        V(lambda: nc.vector.tensor_reduce(out=r[:, 40:41], in_=r[:, 0:4], axis=AX.X, op=ALU.max), r=[r], w=[r])
        V(lambda: nc.vector.tensor_scalar(out=r[:, 41:42], in0=r[:, 40:41], scalar1=-1.0, scalar2=None, op0=ALU.mult), r=[r], w=[r])
        A(lambda: nc.scalar.activation(out=r[:, 36:40], in_=r[:, 0:4], func=AF.Exp, bias=r[:, 41:42], scale=1.0, accum_out=r[:, 42:43]), r=[r], w=[r])
        V(lambda: nc.vector.reciprocal(out=r[:, 43:44], in_=r[:, 42:43]), r=[r], w=[r])
        V(lambda: nc.vector.tensor_scalar(out=r[:, 44:48], in0=r[:, 0:4], scalar1=r[:, 40:41], scalar2=None, op0=ALU.is_equal), r=[r], w=[r])
        V(lambda: nc.vector.tensor_scalar(out=r[:, 44:48], in0=r[:, 44:48], scalar1=1.0, scalar2=1e30, op0=ALU.subtract, op1=ALU.mult), r=[r], w=[r])
        V(lambda: nc.vector.tensor_tensor(out=r[:, 48:80].rearrange("p (g e) -> p g e", g=4), in0=r[:, 4:36].rearrange("p (g e) -> p g e", g=4),
                                          in1=r[:, 44:48].unsqueeze(2).broadcast_to([128, 4, 8]), op=ALU.add), r=[r], w=[r])
        V(lambda: nc.vector.max(out=r[:, 80:88], in_=r[:, 48:80]), r=[r], w=[r])
        V(lambda: nc.vector.tensor_tensor(out=r[:, 88:89], in0=r[:, 81:82], in1=r[:, 80:81], op=ALU.subtract), r=[r], w=[r])
        A(lambda: nc.scalar.activation(out=r[:, 89:90], in_=r[:, 88:89], func=AF.Exp), r=[r], w=[r])
        V(lambda: nc.vector.tensor_scalar(out=r[:, 90:91], in0=r[:, 89:90], scalar1=1.0, scalar2=None, op0=ALU.add), r=[r], w=[r])
        V(lambda: nc.vector.reciprocal(out=r[:, 91:92], in_=r[:, 90:91]), r=[r], w=[r])
        V(lambda: nc.vector.tensor_tensor(out=r[:, 92:93], in0=r[:, 91:92], in1=r[:, 43:44], op=ALU.mult), r=[r], w=[r])
        V(lambda: nc.vector.tensor_tensor(out=r[:, 93:94], in0=r[:, 92:93], in1=r[:, 89:90], op=ALU.mult), r=[r], w=[r])
        V(lambda: nc.vector.tensor_scalar(out=r[:, 96:128], in0=r[:, 48:80], scalar1=r[:, 80:81], scalar2=None, op0=ALU.is_equal), r=[r], w=[r])
        V(lambda: nc.vector.tensor_scalar(out=r[:, 128:160], in0=r[:, 48:80], scalar1=r[:, 81:82], scalar2=None, op0=ALU.is_equal), r=[r], w=[r])
        yield
        yield from idle(5 * bgslack)
        if t == 0:
            V(lambda: nc.vector.scalar_tensor_tensor(out=Ab[:, :], in0=r[:, 96:128], scalar=valid0[:, 0:1], in1=r[:, 128:160],
                                                     op0=ALU.mult, op1=ALU.add), r=[r, valid0], w=[Ab])
            V(lambda: nc.vector.tensor_scalar(out=Ab[:, :], in0=Ab[:, :], scalar1=valid0[:, 0:1], scalar2=1.0, op0=ALU.mult, op1=ALU.min), r=[Ab, valid0], w=[Ab])
        else:
            V(lambda: nc.vector.tensor_tensor(out=Ab[:, :], in0=r[:, 96:128], in1=r[:, 128:160], op=ALU.add), r=[r], w=[Ab])
        PE(lambda: nc.tensor.matmul(psS[:, 64:96], lhsT=ltb[:, :], rhs=Ab[:, :], start=True, stop=True), r=[ltb, Ab], w=[psS], inc=False)
        PE(lambda: nc.tensor.matmul(psS[:, 128:160], lhsT=onesb[:, :], rhs=Ab[:, :], start=True, stop=True), r=[onesb, Ab], w=[psS])
        V(lambda: nc.vector.tensor_tensor(out=r[:, 48:80], in0=psS[:, 64:96], in1=base[:, :], op=ALU.add), r=[psS, base, r], w=[r])
        V(lambda: nc.vector.tensor_tensor(out=base[:, :], in0=psS[:, 128:160], in1=base[:, :], op=ALU.add), r=[psS], w=[base])
        V(lambda: nc.vector.tensor_tensor(out=r[:, 0:32], in0=r[:, 48:80], in1=slotc[:, 32:64], op=ALU.is_ge), r=[r, slotc], w=[r])
        V(lambda: nc.vector.scalar_tensor_tensor(out=r[:, 48:80], in0=r[:, 0:32], scalar=1e7, in1=r[:, 48:80], op0=ALU.mult, op1=ALU.add), r=[r], w=[r])
        V(lambda: nc.vector.scalar_tensor_tensor(out=r[:, 0:32], in0=r[:, 96:128], scalar=1.0, in1=r[:, 48:80],
                                                 op0=ALU.mult, op1=ALU.mult, accum_out=r[:, 36:37]), r=[r], w=[r])
        V(lambda: nc.vector.scalar_tensor_tensor(out=r[:, 0:32], in0=r[:, 128:160], scalar=1.0, in1=r[:, 48:80],
                                                 op0=ALU.mult, op1=ALU.mult, accum_out=r[:, 37:38]), r=[r], w=[r])
        if t == 0:
            V(lambda: nc.vector.tensor_scalar(out=r[:, 36:38], in0=r[:, 36:38], scalar1=valid0[:, 1:2], scalar2=None, op0=ALU.add), r=[r, valid0], w=[r])
        V(lambda: nc.vector.tensor_scalar(out=r[:, 36:38], in0=r[:, 36:38], scalar1=3e7, scalar2=None, op0=ALU.min), r=[r], w=[r])
        yield
        di = rb["desti"].get()
        V(lambda: nc.vector.tensor_copy(out=di[:, :], in_=r[:, 36:38]), r=[r], w=[di])
        for k in range(2):
            V(lambda k=k: nc.vector.tensor_copy(out=x2[:, k, 512:513], in_=retc[:, t, k:k + 1]), r=[retc], w=[x2])
            V(lambda k=k: nc.vector.tensor_copy(out=x2[:, k, 513:514].bitcast(F32), in_=r[:, 92 + k:93 + k]), r=[r], w=[x2])
        for k in range(2):
            DMA("pool", lambda e, k=k: e.indirect_dma_start(out=xs_d[l][:, :], out_offset=bass.IndirectOffsetOnAxis(ap=di[:, k:k + 1], axis=0),
                                                             in_=x2[:, k, :], in_offset=None, bounds_check=reg_xs, oob_is_err=False),
                r=[x2, di, b_xsi[l]], s=[b_xs[l]])

    def pass1():
        common_bufs()
        xinit = AT("xinit", [128, NSLOT // 128, 2], I32)
        ld("sp", xinit, xinit[:, :, :], I["c_xinit"].ap())
        NPP = NSLOT // 128
        for l in range(2):
            for j in range(8):
                DMA("sp", lambda e, l=l, j=j: e.dma_start(
                    out=xs_d[l].ap().rearrange("(p n) w -> p n w", p=128)[:, j * (NPP // 8):(j + 1) * (NPP // 8), 512:514],
                    in_=xinit[:, j * (NPP // 8):(j + 1) * (NPP // 8), :]), r=[xinit], s=[b_xsi[l]])
        gt = AT("g1", [128, 3, D], F32)
        ld("sp", gt, gt[:, 0:2, :], I["gains"].ap()[:, G_ANORM:G_ASCALE + 1, :])
        ld("sp", gt, gt[:, 2, :], I["gains"].ap()[:, G_FFN0, :])
        poolc = AT("poolc", [128, 3, 4, 128], BF16)
        ld("pool", poolc, poolc[:, :, :, :], I["c_pool"].ap().rearrange("a g i o -> i a g o"))
        aw0 = AT("aw0", [128, 4, 2, 256], F32)
        for g in range(4):
            DMA("sp", lambda e, g=g: e.dma_start(out=aw0[:, g, :, :], in_=I["a_w"].ap()[g].rearrange("(kc p) f -> p kc f", p=128)), s=[aw0])
        aw = AT("aw", [128, 4, 2, 256], BF16)
        for g in range(4):
            V(lambda g=g: nc.vector.tensor_tensor(out=aw[:, g, :, :], in0=aw0[:, g, :, :],
                                                  in1=gt[:, 1, g * 256:(g + 1) * 256].unsqueeze(1).broadcast_to([128, 2, 256]), op=ALU.mult),
              r=[aw0, gt], w=[aw])
        V(lambda: nc.vector.tensor_copy(out=base[:, :], in_=slotc[:, 0:32]), r=[slotc], w=[base])
        h0 = [AT(f"h0_{i}", [128, D], F32) for i in range(3)]
        hnb = [AT(f"hnb{i}", [128, D], BF16) for i in range(2)]
        plT = AT("plT", [128, 8, 128], BF16)
        tmpf = AT("tmpf", [128, D], F32)
        rb = route_bufs(gt, gt[:, 2, :])

        hnb3 = hnb + [AT("hnb2", [128, D], BF16)]
        plT2 = [plT, AT("plT_b", [128, 8, 128], BF16)]
        tmpf2 = [tmpf, AT("tmpf_b", [128, D], F32)]

        def p1_tile(t):
            hb = h0[t % 3]
            if t == 0:
                V(lambda: nc.vector.memset(hb[:, :], 0.0), w=[hb])
                DMA("sp", lambda e: e.dma_start(out=hb[112:128, :], in_=I["meta_tokens"].ap()), w=[hb])
            else:
                ld("sp", hb, hb[:, :], I["x"].ap()[(t - 1) * 128:t * 128, :])
            yield
            s = rstd_of(hb, hb[:, :], D)
            hn = hnb3[t % 3]
            hp = hnb3[(t - 1) % 3]
            pl = plT2[t % 2]
            tm = tmpf2[t % 2]
            V(lambda: nc.vector.scalar_tensor_tensor(out=hn[:, :], in0=hb[:, :], scalar=s[:, 3:4], in1=gt[:, 0, :],
                                                     op0=ALU.mult, op1=ALU.mult), r=[hb, s, gt], w=[hn])
            yield
            for cc in range(8):
                g = cc // 2
                ps = PS[0] if cc < 4 else PS[1]
                o = ps[:, (cc % 4) * 128:(cc % 4 + 1) * 128]
                if t == 0:
                    PE(lambda cc=cc, o=o, g=g: nc.tensor.matmul(o, lhsT=hn[:, cc * 128:(cc + 1) * 128], rhs=poolc[:, 2, g, :], start=True, stop=True),
                       r=[hn, poolc], w=[ps], inc=(cc % 4 == 3))
                else:
                    PE(lambda cc=cc, o=o, g=g: nc.tensor.matmul(o, lhsT=hn[:, cc * 128:(cc + 1) * 128], rhs=poolc[:, 0, g, :], start=True, stop=False),
                       r=[hn, poolc], w=[ps], inc=False)
                    PE(lambda cc=cc, o=o, g=g: nc.tensor.matmul(o, lhsT=hp[:, cc * 128:(cc + 1) * 128], rhs=poolc[:, 1, g, :], start=False, stop=True),
                       r=[hp, poolc], w=[ps], inc=(cc % 4 == 3))
            A(lambda: nc.scalar.activation(out=pl[:, 0:4, :], in_=PS[0][:, :].rearrange("p (c k) -> p c k", c=4), func=AF.Copy), r=[PS[0]], w=[pl])
            A(lambda: nc.scalar.activation(out=pl[:, 4:8, :], in_=PS[1][:, :].rearrange("p (c k) -> p c k", c=4), func=AF.Copy), r=[PS[1]], w=[pl])
            yield
            for g in range(4):
                ps = PS[2] if g < 2 else PS[3]
                for kc in range(2):
                    PE(lambda g=g, kc=kc, ps=ps: nc.tensor.matmul(ps[:, (g % 2) * 256:(g % 2 + 1) * 256], lhsT=pl[:, 2 * g + kc, :], rhs=aw[:, g, kc, :],
                                                                  start=(kc == 0), stop=(kc == 1)), r=[pl, aw], w=[ps], inc=(g % 2 == 1 and kc == 1))
            for hf in range(2):
                V(lambda hf=hf: nc.vector.tensor_tensor(out=hb[:, hf * 512:(hf + 1) * 512], in0=PS[2 + hf][:, :], in1=hb[:, hf * 512:(hf + 1) * 512],
                                                        op=ALU.add), r=[PS[2 + hf]], w=[hb])
            yield
            DMA("pool", lambda e: e.dma_start(out=h_d[t * 128:(t + 1) * 128, :], in_=hb[:, :]), r=[hb], w=[b_h[t]])
            yield from route_and_scatter(0, t, hb, hb[:, :], PS[4], PS[5], PS[6], rb)

        pipeline([(lambda t=t: p1_tile(t)) for t in range(NT)], depth=3, skew=3)

    def expert_loop(l):
        NWB = 4
        NXB = 4 * ST
        wgt = [AT(f"wg{i}", [128, 2048], BF16) for i in range(NWB)]
        wut = [AT(f"wu{i}", [128, 2048], BF16) for i in range(NWB)]
        wdt = [AT(f"wd{i}", [128, 2048], BF16) for i in range(NWB)]
        xt = [AT(f"xt{i}", [128, XW], I32) for i in range(NXB)]
        xeT = [AT(f"xeT{i}", [128, 8, CAP], BF16) for i in range(2)]
        sgt = [AT(f"sg{i}", [128, CAP], F32) for i in range(2)]
        cnt_t = AT("cnt_dbg", [128, NE], F32)
        hact = [AT(f"hact{i}", [128, 2, CAP], BF16) for i in range(2)]
        ysb = Rot("ysb", [128, D], F32, 3)

        def ex_load(e):
            i = e % NWB
            ld("pool", wgt[i], wgt[i][:, :], I["w_gate"].ap()[l, e].rearrange("(p c) f -> p (c f)", c=8))
            ld("pool", wut[i], wut[i][:, :], I["w_up"].ap()[l, e].rearrange("(p c) f -> p (c f)", c=8))
            ld("pool", wdt[i], wdt[i][:, :], I["w_down"].ap()[l, e].rearrange("(p c) d -> p (c d)", c=2))
            for s_ in range(ST):
                x = xt[(e * ST + s_) % NXB]
                r0 = (e * ST + s_) * 128
                ld("sp", x, x[:, :], xs_d[l][r0:r0 + 128, :], r=[b_xs[l]])

        def ex_compute(e):
            if e + 2 < NE:
                ex_load(e + 2)
            i = e % NWB
            xe = xeT[e % 2]
            ha = hact[e % 2]
            for s_ in range(ST):
                x = xt[(e * ST + s_) % NXB]
                xv = x[:, 0:512].bitcast(BF16).rearrange("s (p c) -> s c p", c=8)
                ps = PS[s_ % 2]
                pv = ps[:, :].bitcast(BF16).rearrange("p (c k) -> p c k", c=8)
                for c in range(8):
                    PE(lambda c=c, pv=pv, xv=xv: nc.tensor.transpose(out=pv[:, c, :], in_=xv[:, c, :], identity=ident[:, :]),
                       r=[x, ident], w=[ps], inc=(c == 7))
                if s_ % 2 == 0:
                    A(lambda pv=pv, s_=s_: nc.scalar.activation(out=xe[:, :, s_ * 128:(s_ + 1) * 128], in_=pv, func=AF.Copy), r=[ps], w=[xe])
                else:
                    V(lambda pv=pv, s_=s_: nc.vector.tensor_copy(out=xe[:, :, s_ * 128:(s_ + 1) * 128], in_=pv), r=[ps], w=[xe])
                yield
            wgv = wgt[i][:, :].rearrange("p (c f two) -> p c two f", c=8, two=2)
            wuv = wut[i][:, :].rearrange("p (c f two) -> p c two f", c=8, two=2)
            wdv = wdt[i][:, :].rearrange("p (c d) -> p c d", c=2)
            for fc in range(2):
                for wv, wtl, ps in ((wgv, wgt[i], PS[2 + fc]), (wuv, wut[i], PS[4 + fc])):
                    for c in range(8):
                        PE(lambda c=c, wv=wv, ps=ps, fc=fc: nc.tensor.matmul(ps[:, 0:CAP], lhsT=wv[:, c, fc, :], rhs=xe[:, c, :], start=(c == 0), stop=(c == 7)),
                           r=[wtl, xe], w=[ps], inc=(c == 7))
                A(lambda fc=fc: nc.scalar.activation(out=sgt[fc][:, :], in_=PS[2 + fc][:, 0:CAP], func=AF.Silu), r=[PS[2 + fc]], w=[sgt[fc]])
                V(lambda fc=fc: nc.vector.tensor_tensor(out=ha[:, fc, :], in0=sgt[fc][:, :], in1=PS[4 + fc][:, 0:CAP], op=ALU.mult),
                  r=[sgt[fc], PS[4 + fc]], w=[ha])
                yield
            for s_ in range(ST):
                x = xt[(e * ST + s_) % NXB]
                for hf in range(2):
                    ps = PS[6 + hf]
                    for fc in range(2):
                        PE(lambda fc=fc, hf=hf, ps=ps, s_=s_: nc.tensor.matmul(ps[:, :], lhsT=ha[:, fc, s_ * 128:(s_ + 1) * 128], rhs=wdv[:, fc, hf * 512:(hf + 1) * 512],
                                                                              start=(fc == 0), stop=(fc == 1)), r=[ha, wdt[i]], w=[ps], inc=(fc == 1))
                y = ysb.get()
                gate_ap = x[:, 513:514].bitcast(F32)
                A(lambda y=y, gate_ap=gate_ap: nc.scalar.activation(out=y[:, 0:512], in_=PS[6][:, :], func=AF.Copy, scale=gate_ap), r=[PS[6], x], w=[y])
                V(lambda y=y, gate_ap=gate_ap: nc.vector.tensor_scalar(out=y[:, 512:1024], in0=PS[7][:, :], scalar1=gate_ap, scalar2=None, op0=ALU.mult), r=[PS[7], x], w=[y])
                DMA("pool", lambda e_, y=y, x=x: e_.indirect_dma_start(out=yk_d[l][:, :], out_offset=bass.IndirectOffsetOnAxis(ap=x[:, 512:513], axis=0),
                                                                        in_=y[:, :], in_offset=None, bounds_check=reg_yk, oob_is_err=False),
                    r=[y, x], s=[b_yk[l]])
                yield

        ex_load(0)
        ex_load(1)
        pipeline([(lambda e=e: ex_compute(e)) for e in range(NE)], depth=2, skew=ST)

    def pass2a():
        common_bufs()
        gk = AT("gk", [128, D], F32)
        ld("sp", gk, gk[:, :], I["gains"].ap()[:, G_KV, :])
        gl = AT("gl", [128, 256], F32)
        ld("sp", gl, gl[:, :], I["kvl_gain"].ap())
        wdkv = AT("wdkv", [128, 8, 384], BF16)
        wsrc = I["w_dkv"].ap().rearrange("(c p) n -> p c n", p=128)
        DMA("pool", lambda e: e.dma_start(out=wdkv[:, :, 0:320], in_=wsrc), w=[wdkv])
        DMA("pool", lambda e: e.dma_start(out=wdkv[:, :, 320:352], in_=wsrc[:, :, 288:320]), s=[wdkv])
        DMA("pool", lambda e: e.dma_start(out=wdkv[:, :, 352:384], in_=wsrc[:, :, 256:288]), s=[wdkv])
        wuk = AT("wuk", [128, 2, D], BF16)
        ld("pool", wuk, wuk[:, :, :], I["w_uk"].ap().rearrange("(rc p) n -> p rc n", p=128))
        wuv = AT("wuv", [128, 2, D], BF16)
        ld("pool", wuv, wuv[:, :, :], I["w_uv"].ap().rearrange("(rc p) n -> p rc n", p=128))
        hA = [AT(f"hA{i}", [128, D], F32) for i in range(3)]
        ykb = [AT(f"ykb{i}", [128, 2, D], F32) for i in range(3)]
        ropec = [AT(f"ropec{i}", [64, 2, 128], F32) for i in range(3)]
        hnk_l = [AT(f"hnk{i}", [128, D], BF16) for i in range(2)]
        hnT_l = [AT(f"hnT{i}", [128, 8, 128], BF16) for i in range(2)]
        clat_l = [AT(f"clat{i}", [128, 256], F32) for i in range(2)]
        ckvn_l = [AT(f"ckvn{i}", [128, 256], BF16) for i in range(2)]
        ckT_l = [AT(f"ckT{i}", [128, 2, 128], BF16) for i in range(2)]
        kT = [AT(f"kT{i}", [128, 8, 128], BF16) for i in range(2)]
        vt = [AT(f"vt{i}", [128, 8, 130], BF16) for i in range(3)]
        krT = [AT(f"krT{i}", [64, 128], BF16) for i in range(2)]
        rtmp_l = [AT(f"rtmp{i}", [64, 2, 128], F32) for i in range(2)]
        for i in range(3):
            V(lambda i=i: nc.vector.memset(vt[i][:, :, 128:130], 1.0), w=[vt[i]])

        def tile(t):
            h = hA[t % 3]
            y = ykb[t % 3]
            hnk, hnT, clat, ckvn, ckT, rtmp = hnk_l[t % 2], hnT_l[t % 2], clat_l[t % 2], ckvn_l[t % 2], ckT_l[t % 2], rtmp_l[t % 2]
            ld("sp", hA[t % 3], hA[t % 3][:, :], h_d[t * 128:(t + 1) * 128, :], r=[b_h[t]])
            ld("sp", ykb[t % 3], ykb[t % 3][:, :, :], yk_d[0][t * 256:(t + 1) * 256, :].rearrange("(p k) d -> p k d", k=2), r=[b_yk[0]])
            ld("sp", ropec[t % 3], ropec[t % 3][:, :, :], I["c_rope"].ap()[:, :, t * 128:(t + 1) * 128].rearrange("a j k -> j a k"))
            yield
            V(lambda: nc.vector.tensor_tensor(out=h[:, :], in0=h[:, :], in1=y[:, 0, :], op=ALU.add), r=[y], w=[h])
            G(lambda: nc.gpsimd.tensor_tensor(out=h[:, :], in0=h[:, :], in1=y[:, 1, :], op=ALU.add), r=[y], w=[h])
            if t == 0:
                V(lambda: nc.vector.memset(h[0:112, :], 0.0), w=[h])
            DMA("pool", lambda e: e.dma_start(out=h_d[t * 128:(t + 1) * 128, :], in_=h[:, :]), r=[h], w=[b_h[t]])
            s = rstd_of(h, h[:, :], D)
            V(lambda: nc.vector.scalar_tensor_tensor(out=hnk[:, :], in0=h[:, :], scalar=s[:, 3:4], in1=gk[:, :], op0=ALU.mult, op1=ALU.mult),
              r=[h, s, gk], w=[hnk])
            yield
            pv = PS[0][:, :].bitcast(BF16).rearrange("p (c k) -> p c k", c=8)
            for c in range(8):
                PE(lambda c=c: nc.tensor.transpose(out=pv[:, c, :], in_=hnk[:, c * 128:(c + 1) * 128], identity=ident[:, :]),
                   r=[hnk, ident], w=[PS[0]], inc=(c == 7))
            A(lambda: nc.scalar.activation(out=hnT[:, :, :], in_=pv, func=AF.Copy), r=[PS[0]], w=[hnT])
            for c in range(8):
                PE(lambda c=c: nc.tensor.matmul(PS[1][:, 0:256], lhsT=hnT[:, c, :], rhs=wdkv[:, c, 0:256], start=(c == 0), stop=(c == 7)),
                   r=[hnT, wdkv], w=[PS[1]], inc=(c == 7))
            yield
            for j in range(2):
                for c in range(8):
                    PE(lambda c=c, j=j: nc.tensor.matmul(PS[2][0:64, j * 128:(j + 1) * 128], lhsT=wdkv[:, c, 256 + 64 * j:320 + 64 * j], rhs=hnT[:, c, :],
                                                         start=(c == 0), stop=(c == 7)), r=[hnT, wdkv], w=[PS[2]], inc=(c == 7))
            rc_ = ropec[t % 3]
            V(lambda: nc.vector.tensor_tensor(out=rtmp[:, :, :], in0=PS[2][0:64, 0:256].rearrange("p (a k) -> p a k", a=2), in1=rc_[:, :, :], op=ALU.mult),
              r=[PS[2], rc_], w=[rtmp])
            kr = krT[t % 2]
            V(lambda: nc.vector.tensor_tensor(out=kr[:, :], in0=rtmp[:, 0, :], in1=rtmp[:, 1, :], op=ALU.add), r=[rtmp], w=[kr])
            DMA("pool", lambda e: e.dma_start(out=kr_d.ap()[:, t, :], in_=kr[:, :]), r=[kr], s=[b_kv[t]])
            yield
            A(lambda: nc.scalar.activation(out=clat[:, :], in_=PS[1][:, 0:256], func=AF.Copy), r=[PS[1]], w=[clat])
            s2 = rstd_of(clat, clat[:, :], 256)
            V(lambda: nc.vector.scalar_tensor_tensor(out=ckvn[:, :], in0=clat[:, :], scalar=s2[:, 3:4], in1=gl[:, :], op0=ALU.mult, op1=ALU.mult),
              r=[clat, s2, gl], w=[ckvn])
            pv3 = PS[3][:, 0:128].bitcast(BF16).rearrange("p (c k) -> p c k", c=2)
            for c in range(2):
                PE(lambda c=c: nc.tensor.transpose(out=pv3[:, c, :], in_=ckvn[:, c * 128:(c + 1) * 128], identity=ident[:, :]),
                   r=[ckvn, ident], w=[PS[3]], inc=(c == 1))
            V(lambda: nc.vector.tensor_copy(out=ckT[:, :, :], in_=pv3), r=[PS[3]], w=[ckT])
            yield
            for hh in range(8):
                ps = PS[4 + hh // 4]
                for rc in range(2):
                    PE(lambda hh=hh, rc=rc, ps=ps: nc.tensor.matmul(ps[:, (hh % 4) * 128:(hh % 4 + 1) * 128], lhsT=wuk[:, rc, hh * 128:(hh + 1) * 128], rhs=ckT[:, rc, :],
                                                                    start=(rc == 0), stop=(rc == 1)), r=[wuk, ckT], w=[ps], inc=(hh % 4 == 3 and rc == 1))
            k_ = kT[t % 2]
            A(lambda: nc.scalar.activation(out=k_[:, 0:4, :], in_=PS[4][:, :].rearrange("p (c k) -> p c k", c=4), func=AF.Copy), r=[PS[4]], w=[k_])
            V(lambda: nc.vector.tensor_copy(out=k_[:, 4:8, :], in_=PS[5][:, :].rearrange("p (c k) -> p c k", c=4)), r=[PS[5]], w=[k_])
            DMA("pool", lambda e: e.dma_start(out=kt_d.ap()[:, :, t, :].rearrange("h p k -> p h k"), in_=k_[:, :, :]), r=[k_], s=[b_kv[t]])
            yield
            for hf in range(2):
                for rc in range(2):
                    PE(lambda hf=hf, rc=rc: nc.tensor.matmul(PS[6 + hf][:, :], lhsT=ckT[:, rc, :], rhs=wuv[:, rc, hf * 512:(hf + 1) * 512],
                                                             start=(rc == 0), stop=(rc == 1)), r=[ckT, wuv], w=[PS[6 + hf]], inc=(rc == 1))
            v_ = vt[t % 3]
            A(lambda: nc.scalar.activation(out=v_[:, 0:4, 0:128], in_=PS[6][:, :].rearrange("p (c k) -> p c k", c=4), func=AF.Copy), r=[PS[6]], w=[v_])
            V(lambda: nc.vector.tensor_copy(out=v_[:, 4:8, 0:128], in_=PS[7][:, :].rearrange("p (c k) -> p c k", c=4)), r=[PS[7]], w=[v_])
            if t == 0:
                V(lambda: nc.vector.memset(v_[0:112, :, :], 0.0), w=[v_])
            DMA("pool", lambda e: e.dma_start(out=vt_d.ap()[:, :, t, :].rearrange("h p n -> p h n"), in_=v_[:, :, :]), r=[v_], s=[b_kv[t]])
            if t == 0:
                V(lambda: nc.vector.memset(v_[:, :, 128:130], 1.0), w=[v_])

        pipeline([(lambda t=t: tile(t)) for t in range(NT)], depth=3, skew=2)

    def pass2b():
        common_bufs()
        gb = AT("gb", [128, 2, D], F32)
        ld("sp", gb, gb[:, 0, :], I["gains"].ap()[:, G_B, :])
        ld("sp", gb, gb[:, 1, :], I["gains"].ap()[:, G_FFN1, :])
        gq = AT("gq", [128, 384], F32)
        ld("sp", gq, gq[:, :], I["qn_gain"].ap())
        wdq = AT("wdq", [128, 8, 384], BF16)
        ld("pool", wdq, wdq[:, :, :], I["b_w_dq"].ap().rearrange("(c p) r -> p c r", p=128))
        wuqn = AT("wuqn", [128, 3, 8, 128], BF16)
        usrc = I["b_w_uq"].ap().rearrange("(rc p) h d -> p rc h d", p=128)
        wuqr = AT("wuqr", [128, 3, 8, 128], BF16)
        wuqs = AT("wuqs", [128, 3, 8, 128], BF16)
        for rc in range(3):
            DMA("pool", lambda e, rc=rc: e.dma_start(out=wuqn[:, rc, :, :], in_=usrc[:, rc, :, 0:128]), s=[wuqn])
            for dup in range(2):
                o = 64 * dup
                DMA("pool", lambda e, rc=rc, o=o: e.dma_start(out=wuqr[:, rc, :, o:o + 64], in_=usrc[:, rc, :, 128:192]), s=[wuqr])
                DMA("pool", lambda e, rc=rc, o=o: e.dma_start(out=wuqs[:, rc, :, o:o + 32], in_=usrc[:, rc, :, 160:192]), s=[wuqs])
                DMA("pool", lambda e, rc=rc, o=o: e.dma_start(out=wuqs[:, rc, :, o + 32:o + 64], in_=usrc[:, rc, :, 128:160]), s=[wuqs])
        wo = AT("wo", [128, 8, D], BF16)
        ld("pool", wo, wo[:, :, :], I["b_w_o"].ap().rearrange("(hh p) d -> p hh d", p=128))
        V(lambda: nc.vector.tensor_copy(out=base[:, :], in_=slotc[:, 0:32]), r=[slotc], w=[base])
        hqa = Rot("hqa", [128, D], F32, 2)
        hqe = Rot("hqe", [128, D], F32, 2)
        hnq = AT("hnq", [128, D], BF16)
        hnT = AT("hnT", [128, 8, 128], BF16)
        cq = AT("cq", [128, 384], F32)
        cqn = AT("cqn", [128, 384], BF16)
        cqT = [AT(f"cqT{i}", [128, 3, QC * 128], BF16) for i in range(2)]
        qT = [AT(f"qT{i}", [128, 8, QC * 128], BF16) for i in range(2)]
        qrT = [AT(f"qrT{i}", [128, 8, QC * 128], BF16) for i in range(2)]
        ropeq = AT("ropeq", [128, 2, QC * 128], F32)
        rq = AT("rq", [128, 2, QC * 128], F32)
        krc = AT("krc", [128, NT * 128], BF16)
        krc_b = [Buf() for _ in range(8)]
        Kh = [AT(f"Kh{i}", [128, NT * 128], BF16) for i in range(2)]
        Vh = [AT(f"Vh{i}", [128, NT, 130], BF16) for i in range(2)]
        pT = Rot("pT", [128, QC * 128], BF16, 6)
        rec = Rot("rec", [128, 4], F32, 2)
        osb = [AT(f"osb{i}", [128, QC, D], BF16) for i in range(2)]
        oT = AT("oT", [128, 8, 128], BF16)
        rb = route_bufs(gb, gb[:, 1, :], n=1)
        print("p2b arena words used", ar["off"], "of", ARENA_W)

        def kv_load(c, hh):
            nk = 4 * c + 5
            i = (c * 8 + hh) % 2
            deps = [b_kv[t] for t in range(nk)]
            ld("sp", Kh[i], Kh[i][:, 0:nk * 128], kt_d.ap()[hh, :, 0:nk, :].rearrange("p t k -> p (t k)"), r=deps)
            ld("sp", Vh[i], Vh[i][:, 0:nk, :], vt_d.ap()[hh, :, 0:nk, :], r=deps)

        def qpath(c):
            cp = c % 2
            t0 = 4 * c + 1
            gid0 = t0 * 128
            ta, tb = (0, 5) if c == 0 else (4 * c + 1, 4 * c + 5)
            for dup in range(2):
                DMA("sp", lambda e, dup=dup: e.dma_start(out=krc[64 * dup:64 * dup + 64, ta * 128:tb * 128],
                                                         in_=kr_d.ap()[:, ta:tb, :].rearrange("j t k -> j (t k)")),
                    r=[b_kv[t] for t in range(ta, tb)], s=[krc_b[c]])
                DMA("sp", lambda e, dup=dup: e.dma_start(out=ropeq[64 * dup:64 * dup + 64, :, :],
                                                         in_=I["c_rope"].ap()[:, :, gid0:gid0 + QC * 128].rearrange("a j k -> j a k")), s=[ropeq])
            for j in range(QC):
                t = t0 + j
                h = hqa.get()
                ld("sp", h, h[:, :], h_d[t * 128:(t + 1) * 128, :], r=[b_h[t]])
                s = rstd_of(h, h[:, :], D)
                V(lambda h=h, s=s: nc.vector.scalar_tensor_tensor(out=hnq[:, :], in0=h[:, :], scalar=s[:, 3:4], in1=gb[:, 0, :], op0=ALU.mult, op1=ALU.mult),
                  r=[h, s, gb], w=[hnq])
                yield
                yield from idle(5)
                pv = PS[6][:, :].bitcast(BF16).rearrange("p (c k) -> p c k", c=8)
                for cc in range(8):
                    PE(lambda cc=cc: nc.tensor.transpose(out=pv[:, cc, :], in_=hnq[:, cc * 128:(cc + 1) * 128], identity=ident[:, :]),
                       r=[hnq, ident], w=[PS[6]], inc=(cc == 7))
                A(lambda: nc.scalar.activation(out=hnT[:, :, :], in_=pv, func=AF.Copy), r=[PS[6]], w=[hnT])
                yield
                yield from idle(2)
                for cc in range(8):
                    PE(lambda cc=cc: nc.tensor.matmul(PS[7][:, 0:384], lhsT=hnT[:, cc, :], rhs=wdq[:, cc, :], start=(cc == 0), stop=(cc == 7)),
                       r=[hnT, wdq], w=[PS[7]], inc=(cc == 7))
                A(lambda: nc.scalar.activation(out=cq[:, :], in_=PS[7][:, 0:384], func=AF.Copy), r=[PS[7]], w=[cq])
                yield
                s2 = rstd_of(cq, cq[:, :], 384)
                V(lambda s2=s2: nc.vector.scalar_tensor_tensor(out=cqn[:, :], in0=cq[:, :], scalar=s2[:, 3:4], in1=gq[:, :], op0=ALU.mult, op1=ALU.mult),
                  r=[cq, s2, gq], w=[cqn])
                yield
                yield from idle(5)
                pv3 = PS[6][:, 0:192].bitcast(BF16).rearrange("p (c k) -> p c k", c=3)
                for rc in range(3):
                    PE(lambda rc=rc: nc.tensor.transpose(out=pv3[:, rc, :], in_=cqn[:, rc * 128:(rc + 1) * 128], identity=ident[:, :]),
                       r=[cqn, ident], w=[PS[6]], inc=(rc == 2))
                V(lambda j=j: nc.vector.tensor_copy(out=cqT[cp][:, :, j * 128:(j + 1) * 128], in_=pv3), r=[PS[6]], w=[cqT[cp]])
                yield
            for hh in range(8):
                for rc in range(3):
                    PE(lambda hh=hh, rc=rc: nc.tensor.matmul(PS[6][:, :], lhsT=wuqn[:, rc, hh, :], rhs=cqT[cp][:, rc, :], start=(rc == 0), stop=(rc == 2)),
                       r=[wuqn, cqT[cp]], w=[PS[6]], inc=(rc == 2))
                A(lambda hh=hh: nc.scalar.activation(out=qT[cp][:, hh, :], in_=PS[6][:, :], func=AF.Copy), r=[PS[6]], w=[qT[cp]])
                yield
                for rc in range(3):
                    PE(lambda hh=hh, rc=rc: nc.tensor.matmul(PS[7][:, :], lhsT=wuqr[:, rc, hh, :], rhs=cqT[cp][:, rc, :], start=(rc == 0), stop=(rc == 2)),
                       r=[wuqr, cqT[cp]], w=[PS[7]], inc=(rc == 2))
                V(lambda: nc.vector.tensor_tensor(out=rq[:, 0, :], in0=PS[7][:, :], in1=ropeq[:, 0, :], op=ALU.mult), r=[PS[7], ropeq], w=[rq])
                yield
                for rc in range(3):
                    PE(lambda hh=hh, rc=rc: nc.tensor.matmul(PS[6][:, :], lhsT=wuqs[:, rc, hh, :], rhs=cqT[cp][:, rc, :], start=(rc == 0), stop=(rc == 2)),
                       r=[wuqs, cqT[cp]], w=[PS[6]], inc=(rc == 2))
                V(lambda: nc.vector.tensor_tensor(out=rq[:, 1, :], in0=PS[6][:, :], in1=ropeq[:, 1, :], op=ALU.mult), r=[PS[6], ropeq], w=[rq])
                G(lambda hh=hh: nc.gpsimd.tensor_tensor(out=qrT[cp][:, hh, :], in0=rq[:, 0, :], in1=rq[:, 1, :], op=ALU.add), r=[rq], w=[qrT[cp]])
                yield

        def attention(c):
            cp = c % 2
            nk = 4 * c + 5
            t0 = 4 * c + 1
            steps = [(hh, kt) for hh in range(8) for kt in range(nk)]
            pend = {}

            def emit_S_pair(i0_, i1_):
                info = []
                pbase = 2 * ((i0_ // 2) % 2)
                for slot, idx in enumerate((i0_, i1_)):
                    hh, kt = steps[idx]
                    if kt == 0 and hh == 0:
                        kv_load(c, 0)
                        kv_load(c, 1)
                    K_ = Kh[(c * 8 + hh) % 2]
                    i0 = max(0, kt - t0)
                    col0 = i0 * 128
                    pss = PS[pbase + slot]
                    PE(lambda K_=K_, kt=kt, hh=hh, col0=col0, pss=pss: nc.tensor.matmul(pss[:, col0:512], lhsT=K_[:, kt * 128:(kt + 1) * 128],
                                                                                        rhs=qT[cp][:, hh, col0:512], start=True, stop=False),
                       r=[K_, qT[cp]], w=[pss], inc=False)
                    info.append((hh, kt, i0, col0, pss))
                for slot, (hh, kt, i0, col0, pss) in enumerate(info):
                    diag = kt >= t0
                    kseg = krc_b[0] if kt < 5 else krc_b[(kt - 1) // 4]
                    r0 = 64 * slot
                    PE(lambda kt=kt, hh=hh, col0=col0, pss=pss, r0=r0: nc.tensor.matmul(pss[:, col0:512], lhsT=krc[r0:r0 + 64, kt * 128:(kt + 1) * 128],
                                                                                        rhs=qrT[cp][r0:r0 + 64, hh, col0:512], start=False, stop=True),
                       r=[kseg, qrT[cp]], w=[pss], inc=(slot == 1))
                for slot, (hh, kt, i0, col0, pss) in enumerate(info):
                    idx = (i0_, i1_)[slot]
                    if kt >= t0:
                        PE(lambda col0=col0, pss=pss: nc.tensor.matmul(pss[:, col0:col0 + 128], lhsT=ident[:, :], rhs=maskb[:, :], start=False, stop=True),
                           r=[ident, maskb], w=[pss])
                    p_ = pT.get()
                    A(lambda col0=col0, pss=pss, p_=p_: nc.scalar.activation(out=p_[:, col0:512], in_=pss[:, col0:512], func=AF.Exp, scale=ATTN_SCALE),
                      r=[pss], w=[p_])
                    pend[idx] = (p_, i0)

            def emit_PV(idx):
                hh, kt = steps[idx]
                p_, i0 = pend.pop(idx)
                V_ = Vh[(c * 8 + hh) % 2]
                PO = [PS[4], PS[5]]
                for qi in range(i0, QC):
                    po = PO[qi // 2]
                    last = (kt == t0 + qi)
                    PE(lambda qi=qi, po=po, last=last: nc.tensor.matmul(po[:, (qi % 2) * 256:(qi % 2) * 256 + 129], lhsT=p_[:, qi * 128:(qi + 1) * 128],
                                                                        rhs=V_[:, kt, 0:129], start=(kt == 0 and qi % 2 == 0), stop=last),
                       r=[p_, V_], w=[po], inc=(qi == QC - 1))
                if kt == nk - 1 and hh + 2 < 8:
                    kv_load(c, hh + 2)
                if kt == nk - 1:
                    rc_ = rec.get()
                    for b2 in range(2):
                        pov = PO[b2][:, :].rearrange("p (i n) -> p i n", i=2)
                        V(lambda b2=b2, pov=pov: nc.vector.reciprocal(out=rc_[:, 2 * b2:2 * b2 + 2].unsqueeze(2), in_=pov[:, :, 128:129]), r=[PO[b2]], w=[rc_])
                        V(lambda b2=b2, pov=pov: nc.vector.tensor_tensor(out=osb[cp][:, 2 * b2:2 * b2 + 2, hh * 128:(hh + 1) * 128], in0=pov[:, :, 0:128],
                                                                         in1=rc_[:, 2 * b2:2 * b2 + 2].unsqueeze(2).broadcast_to([128, 2, 128]), op=ALU.mult),
                          r=[PO[b2], rc_], w=[osb[cp]])

            npair = len(steps) // 2
            emit_S_pair(0, 1)
            emit_S_pair(2, 3)
            for i in range(npair):
                emit_PV(2 * i)
                yield
                emit_PV(2 * i + 1)
                if i + 2 < npair:
                    emit_S_pair(2 * i + 4, 2 * i + 5)
                yield

        def epilogue(c):
            cp = c % 2
            t0 = 4 * c + 1
            for j in range(QC):
                t = t0 + j
                h = hqe.get()
                ld("sp", h, h[:, :], h_d[t * 128:(t + 1) * 128, :], r=[b_h[t]])
                pv = PS[6][:, :].bitcast(BF16).rearrange("p (c k) -> p c k", c=8)
                for cc in range(8):
                    PE(lambda cc=cc, j=j: nc.tensor.transpose(out=pv[:, cc, :], in_=osb[cp][:, j, cc * 128:(cc + 1) * 128], identity=ident[:, :]),
                       r=[osb[cp], ident], w=[PS[6]], inc=(cc == 7))
                A(lambda: nc.scalar.activation(out=oT[:, :, :], in_=pv, func=AF.Copy), r=[PS[6]], w=[oT])
                yield
                for hf in range(2):
                    yield from idle(2)
                    for cc in range(8):
                        PE(lambda cc=cc, hf=hf: nc.tensor.matmul(PS[7][:, :], lhsT=oT[:, cc, :], rhs=wo[:, cc, hf * 512:(hf + 1) * 512], start=(cc == 0), stop=(cc == 7)),
                           r=[oT, wo], w=[PS[7]], inc=(cc == 7))
                    V(lambda hf=hf, h=h: nc.vector.tensor_tensor(out=h[:, hf * 512:(hf + 1) * 512], in0=h[:, hf * 512:(hf + 1) * 512], in1=PS[7][:, :], op=ALU.add),
                      r=[PS[7]], w=[h])
                    yield
                DMA("pool", lambda e, t=t, h=h: e.dma_start(out=h_d[t * 128:(t + 1) * 128, :], in_=h[:, :]), r=[h], w=[b_h[t]])
                yield from route_and_scatter(1, t, h, h[:, :], PS[6], PS[6], PS[7], rb, bgslack=2)

        def drive(main, bgs, n_main, bg_est=150):
            bgs = list(bgs)
            used = 0
            for i, _ in enumerate(main):
                per = max(1, -(-(bg_est - used) // max(1, n_main - i)))
                budget = per
                while budget > 0 and bgs:
                    try:
                        r_ = next(bgs[0])
                        budget -= 1
                        used += 1
                        if r_ == "idle" and per <= 2:
                            break
                    except StopIteration:
                        bgs.pop(0)
            for g in bgs:
                for _ in g:
                    pass

        for _ in qpath(0):
            pass
        nchunks = 1 if stop_after == "p2b_c0" else 8
        for c in range(nchunks):
            bgs = []
            if c > 0:
                bgs.append(epilogue(c - 1))
            if c + 1 < nchunks:
                bgs.append(qpath(c + 1))
            n_main = 8 * (4 * c + 5)
            drive(attention(c), bgs, n_main)
        for _ in epilogue(nchunks - 1):
            pass

    def pass3():
        common_bufs()
        gf = AT("gf", [128, D], F32)
        ld("sp", gf, gf[:, :], I["gains"].ap()[:, G_FINAL, :])
        hA = [AT(f"hA{i}", [128, D], F32) for i in range(3)]
        ykb = [AT(f"ykb{i}", [128, 2, D], F32) for i in range(3)]
        ob = [AT(f"ob{i}", [128, D], F32) for i in range(2)]

        def tile(t):
            h, y, o = hA[t % 3], ykb[t % 3], ob[t % 2]
            ld("sp", h, h[:, :], h_d[t * 128:(t + 1) * 128, :], r=[b_h[t]])
            ld("sp", y, y[:, :, :], yk_d[1][t * 256:(t + 1) * 256, :].rearrange("(p k) d -> p k d", k=2), r=[b_yk[1]])
            yield
            V(lambda: nc.vector.tensor_tensor(out=h[:, :], in0=h[:, :], in1=y[:, 0, :], op=ALU.add), r=[y], w=[h])
            G(lambda: nc.gpsimd.tensor_tensor(out=h[:, :], in0=h[:, :], in1=y[:, 1, :], op=ALU.add), r=[y], w=[h])
            yield
            s = rstd_of(h, h[:, :], D)
            yield
            V(lambda: nc.vector.scalar_tensor_tensor(out=o[:, :], in0=h[:, :], scalar=s[:, 3:4], in1=gf[:, :], op0=ALU.mult, op1=ALU.mult),
              r=[h, s, gf], w=[o])
            DMA("pool", lambda e, t=t, o=o: e.dma_start(out=out_d[(t - 1) * 128:t * 128, :], in_=o[:, :]), r=[o], s=[b_out])

        pipeline([(lambda t=t: tile(t)) for t in range(1, NT)], depth=3, skew=1)

    pass1()
    if stop_after == "p1":
        if "h_scr" in dbg:
            dbg_dram("h_scr", h_d.ap(), [NT * 128, D], F32, b_h)
        return finish(b_h + [b_xs[0]])
    if "counts" in dbg:
        dbg_out("counts0", base, base[:, :], [128, NE])
    phase("e0")
    expert_loop(0)
    if stop_after == "e0":
        if "yk_scr0" in dbg:
            dbg_dram("yk_scr0", yk_d[0].ap(), [NYK, D], F32, [b_yk[0]])
        return finish(b_h + [b_xs[0], b_yk[0]])
    phase("p2a")
    pass2a()
    if stop_after == "p2a":
        dbg_dram("h_scr", h_d.ap(), [NT * 128, D], F32, b_h)
        dbg_dram("kt", kt_d.ap(), [8, 128, NT, 128], BF16, b_kv)
        dbg_dram("kr", kr_d.ap(), [64, NT, 128], BF16, b_kv)
        dbg_dram("vt", vt_d.ap(), [8, 128, NT, 130], BF16, b_kv)
        return finish(b_h + b_kv)
    phase("p2b")
    pass2b()
    if stop_after in ("p2b", "p2b_c0"):
        dbg_dram("h_scr", h_d.ap(), [NT * 128, D], F32, b_h)
        return finish(b_h + [b_xs[1]])
    if "counts" in dbg:
        dbg_out("counts1", base, base[:, :], [128, NE])
    phase("e1")
    expert_loop(1)
    phase("p3")
    pass3()
    return finish([b_out])


_CONSTS = None


def make_in_maps(inp):
    global _CONSTS
    if _CONSTS is None:
        _CONSTS = _consts()
    f = lambda a: np.ascontiguousarray(np.asarray(a, dtype=np.float32))
    rep = lambda v: np.broadcast_to(f(v)[None, :], (128, f(v).shape[0]))
    gains = np.stack([rep(inp["a_norm"][0]), rep(inp["a_scale"][0]), rep(inp["ffn_norm"][0]), rep(inp["ffn_norm"][1]),
                      rep(inp["kv_norm"]), rep(inp["b_norm"][0]), rep(inp["final_norm"])], axis=1)
    router_w = np.concatenate([f(inp["router_g"]), f(inp["router_e"])], axis=2)
    rb = np.concatenate([f(inp["router_g_bias"]), f(inp["router_e_bias"])], axis=1)
    router_b = np.broadcast_to(rb[None], (128, 2, 36))
    shared = {
        "meta_tokens": f(inp["meta_tokens"]), "a_w": f(inp["a_w"][0]), "gains": np.ascontiguousarray(gains),
        "qn_gain": np.ascontiguousarray(rep(inp["b_q_norm"][0])), "kvl_gain": np.ascontiguousarray(rep(inp["kv_lat_norm"])),
        "b_w_dq": f(inp["b_w_dq"][0]), "b_w_uq": f(inp["b_w_uq"][0]), "b_w_o": f(inp["b_w_o"][0]),
        "w_dkv": f(inp["w_dkv"]), "w_uk": f(inp["w_uk"]).reshape(256, 1024), "w_uv": f(inp["w_uv"]).reshape(256, 1024),
        "router_w": np.ascontiguousarray(router_w), "router_b": np.ascontiguousarray(router_b),
        "w_gate": f(inp["w_gate"]), "w_up": f(inp["w_up"]), "w_down": f(inp["w_down"]),
    }
    shared.update(_CONSTS)
    x = f(inp["x"])
    return [dict(shared, x=x[b]) for b in range(8)]


def kernel(**inputs):
    in_maps = make_in_maps(inputs)
    nc = build_program()
    res = run_bass_kernel_spmd(nc, in_maps, core_ids=list(range(8)))
    return np.stack([np.asarray(r["out"], dtype=np.float32) for r in res.results], axis=0)
```

```python
import numpy as np
import concourse.bass as bass
import concourse.mybir as mybir
from concourse.bass_utils import run_bass_kernel_spmd
from concourse.alu_op_type import AluOpType as ALU

F32 = mybir.dt.float32
BF16 = mybir.dt.bfloat16
I32 = mybir.dt.int32
AF = mybir.ActivationFunctionType
AX = mybir.AxisListType

D = 1024
NT = 33
NE = 32
CAP = 512
NSLOT = NE * CAP
ST = CAP // 128
XW = 514
NYK = NT * 128 * 2
EPS = 1e-6
ATTN_SCALE = 192 ** -0.5
OOB = 0x3FFFFFFF
QC = 4


class Buf:
    __slots__ = ("w", "r")

    def __init__(self):
        self.w = {}
        self.r = {}


class KB:
    def __init__(self, nc, n_dma_sems=14):
        self.nc = nc
        self.eng = {"pe": nc.tensor, "act": nc.scalar, "dve": nc.vector, "pool": nc.gpsimd, "sp": nc.sync}
        self.sem = {}
        self.cnt = {}
        for k in ("pe", "act", "dve", "pool"):
            self.sem[k] = nc.alloc_semaphore("s_" + k)
            self.cnt[k] = 0
        self.dpool, self.dcnt, self.dnext = {}, {}, {}
        for q in ("sp", "pool"):
            self.dpool[q] = [nc.alloc_semaphore(f"d_{q}{i}") for i in range(n_dma_sems)]
            self.dcnt[q] = [0] * n_dma_sems
            self.dnext[q] = 0
        self.seen = {k: {} for k in self.eng}
        self.n_wait = 0
        self.n_inst = 0

    def _wait(self, e, tok):
        sem, val, owner = tok
        key = id(sem)
        if self.seen[e].get(key, 0) >= val:
            return
        if owner == e and e == "pe":
            return
        self.eng[e].wait_ge(sem, val)
        self.seen[e][key] = val
        self.n_wait += 1

    def deps(self, e, reads, writes, shared):
        for b in reads:
            for t in b.w.values():
                self._wait(e, t)
        for b in writes:
            for t in b.w.values():
                self._wait(e, t)
            for t in b.r.values():
                self._wait(e, t)
        for b in shared:
            for t in b.r.values():
                self._wait(e, t)

    def _commit(self, tok, reads, writes, shared):
        k = id(tok[0])
        for b in reads:
            b.r[k] = tok
        for b in writes:
            b.w = {k: tok}
            b.r = {}
        for b in shared:
            b.w[k] = tok

    def op(self, e, fn, reads=(), writes=(), inc=True):
        self.deps(e, reads, writes, ())
        ins = fn()
        self.n_inst += 1
        if inc:
            self.cnt[e] += 1
            ins.then_inc(self.sem[e], 1)
            tok = (self.sem[e], self.cnt[e], e)
        else:
            tok = (self.sem[e], self.cnt[e] + 1, e)
        self._commit(tok, reads, writes, ())
        return tok

    def dma(self, q, fn, reads=(), writes=(), shared=()):
        i = self.dnext[q]
        self.dnext[q] = (i + 1) % len(self.dpool[q])
        sem = self.dpool[q][i]
        if self.dcnt[q][i] > 0:
            self._wait(q, (sem, self.dcnt[q][i], "dma"))
        self.deps(q, reads, writes, shared)
        ins = fn(self.eng[q])
        self.n_inst += 1
        self.dcnt[q][i] += 16
        ins.then_inc(sem, 16)
        tok = (sem, self.dcnt[q][i], "dma")
        self._commit(tok, reads, writes, shared)
        return tok

    def finish(self, bufs):
        for b in bufs:
            for t in list(b.w.values()) + list(b.r.values()):
                self._wait("sp", t)


class Tl:
    def __init__(self, nc, name, shape, dtype, psum=False):
        name = "t_" + name
        self.t = nc.alloc_psum_tensor(name, shape, dtype) if psum else nc.alloc_sbuf_tensor(name, shape, dtype)
        self.b = Buf()

    def __getitem__(self, k):
        return self.t[k]


def _consts():
    c = {}
    pool = np.zeros((3, 4, 128, 128), np.float32)
    i = np.arange(128)[:, None]
    o = np.arange(128)[None, :]
    for g, w in enumerate((2, 4, 8, 16)):
        pool[0, g] = ((i > o - w) & (i <= o)) / w - (i == o)
        pool[1, g] = (i - 128 > o - w) / w
        pi, po = i - 112, o - 112
        inw = (pi >= 0) & (po >= 0) & (pi > po - w) & (pi <= po)
        cnt = np.minimum(po + 1, w).astype(np.float32)
        cnt = np.where(po >= 0, cnt, 1.0)
        pool[2, g] = inw / cnt - ((i == o) & (po >= 0))
    c["c_pool"] = pool
    c["c_lt"] = (i < o).astype(np.float32)
    e = np.arange(NE, dtype=np.float32)[None, :]
    c["c_slot"] = np.concatenate([np.broadcast_to(e * CAP, (128, NE)), np.broadcast_to((e + 1) * CAP, (128, NE))], 1).astype(np.float32)
    p = np.arange(128)
    ret = np.zeros((128, NT, 2), np.int32)
    for t in range(NT):
        for k in range(2):
            ret[:, t, k] = (t * 128 + p) * 2 + k
    c["c_ret"] = ret
    v0 = np.zeros((128, 2), np.float32)
    v0[:, 0] = (p >= 112)
    v0[:, 1] = (p < 112) * 1e7
    c["c_valid0"] = v0
    gid = np.arange(NT * 128)
    pos = np.where(gid < 128, gid - 112, 16 + gid - 128).astype(np.float32)
    pos = np.maximum(pos, 0.0).astype(np.float32)
    inv = (np.float32(10000.0) ** (-np.arange(0, 64, 2, dtype=np.float32) / np.float32(64))).astype(np.float32)
    ang = (pos[None, :] * inv[:, None]).astype(np.float32)
    cs, sn = np.cos(ang).astype(np.float32), np.sin(ang).astype(np.float32)
    rope = np.zeros((2, 64, NT * 128), np.float32)
    rope[0, :32], rope[0, 32:] = cs, cs
    rope[1, :32], rope[1, 32:] = -sn, sn
    c["c_rope"] = rope
    c["c_mask"] = np.where(i > o, -30000.0, 0.0).astype(np.float32)
    xi = np.zeros((128, NSLOT // 128, 2), np.int32)
    xi[:, :, 0] = OOB
    c["c_xinit"] = xi
    return c


WEIGHT_SPECS = {
    "x": ([4096, D], F32), "meta_tokens": ([16, D], F32),
    "a_w": ([4, 256, 256], F32), "gains": ([128, 7, D], F32),
    "qn_gain": ([128, 384], F32), "kvl_gain": ([128, 256], F32),
    "b_w_dq": ([D, 384], F32), "b_w_uq": ([384, 8, 192], F32), "b_w_o": ([D, D], F32),
    "w_dkv": ([D, 320], F32), "w_uk": ([256, D], F32), "w_uv": ([256, D], F32),
    "router_w": ([2, D, 36], F32), "router_b": ([128, 2, 36], F32),
    "w_gate": ([2, NE, D, 256], F32), "w_up": ([2, NE, D, 256], F32), "w_down": ([2, NE, 256, D], F32),
    "c_pool": ([3, 4, 128, 128], F32), "c_lt": ([128, 128], F32), "c_slot": ([128, 64], F32),
    "c_ret": ([128, NT, 2], I32), "c_valid0": ([128, 2], F32), "c_rope": ([2, 64, NT * 128], F32),
    "c_mask": ([128, 128], F32), "c_xinit": ([128, NSLOT // 128, 2], I32),
}
G_ANORM, G_ASCALE, G_FFN0, G_FFN1, G_KV, G_B, G_FINAL = range(7)


def build_program(stop_after="all", dbg={}):
    nc = bass.Bass("TRN2", target_bir_lowering=False)
    kb = KB(nc)
    I = {k: nc.dram_tensor(k, s, d, kind="ExternalInput") for k, (s, d) in WEIGHT_SPECS.items()}
    out_d = nc.dram_tensor("out", [4096, D], F32, kind="ExternalOutput")
    b_out = Buf()
    h_d = nc.dram_tensor("h_scr", [NT * 128, D], F32)
    b_h = [Buf() for _ in range(NT)]
    xs_d = [nc.dram_tensor(f"xs_scr{l}", [NSLOT, XW], I32) for l in range(2)]
    b_xs = [Buf(), Buf()]
    b_xsi = [Buf(), Buf()]
    yk_d = [nc.dram_tensor(f"yk_scr{l}", [NYK, D], F32) for l in range(2)]
    b_yk = [Buf(), Buf()]
    kt_d = nc.dram_tensor("kt_scr", [8, 128, NT, 128], BF16)
    kr_d = nc.dram_tensor("kr_scr", [64, NT, 128], BF16)
    vt_d = nc.dram_tensor("vt_scr", [8, 128, NT, 130], BF16)
    b_kv = [Buf() for _ in range(NT)]
    dbg_d = {}
    reg_xs = nc.gpsimd.to_reg(NSLOT - 1)
    reg_yk = nc.gpsimd.to_reg(NYK - 1)

    def T(name, shape, dtype, psum=False):
        return Tl(nc, name, shape, dtype, psum)

    bl = lambda xs: [x.b if isinstance(x, Tl) else x for x in xs]

    def V(fn, r=(), w=(), **k):
        return kb.op("dve", fn, bl(r), bl(w), **k)

    def A(fn, r=(), w=(), **k):
        return kb.op("act", fn, bl(r), bl(w), **k)

    def G(fn, r=(), w=(), **k):
        return kb.op("pool", fn, bl(r), bl(w), **k)

    def PE(fn, r=(), w=(), **k):
        return kb.op("pe", fn, bl(r), bl(w), **k)

    def DMA(q, fn, r=(), w=(), s=()):
        return kb.dma(q, fn, bl(r), bl(w), bl(s))

    def ld(q, dst, dst_ap, src_ap, r=()):
        return DMA(q, lambda e: e.dma_start(out=dst_ap, in_=src_ap), r=r, w=[dst])

    ident = T("ident", [128, 128], BF16)
    identf = T("identf", [128, 128], F32)
    onesb = T("onesb", [128, 128], BF16)
    ltb = T("ltb", [128, 128], BF16)
    slotc = T("slotc", [128, 64], F32)
    retc = T("retc", [128, NT, 2], I32)
    valid0 = T("valid0", [128, 2], F32)
    rbias = T("rbias", [128, 2, 36], F32)
    maskb = T("maskb", [128, 128], BF16)
    base = T("base", [128, NE], F32)
    wr = [T(f"wr{l}", [128, 8, 36], F32) for l in range(2)]
    PS = [T(f"ps{i}", [128, 512], F32, psum=True) for i in range(8)]

    G(lambda: nc.gpsimd.iota(identf[:, :], pattern=[[1, 128]], base=0, channel_multiplier=-1,
                             allow_small_or_imprecise_dtypes=True), w=[identf])
    V(lambda: nc.vector.tensor_single_scalar(out=ident[:, :], in_=identf[:, :], scalar=0.0, op=ALU.is_equal), r=[identf], w=[ident])
    V(lambda: nc.vector.tensor_single_scalar(out=identf[:, :], in_=identf[:, :], scalar=0.0, op=ALU.is_equal), r=[identf], w=[identf])
    V(lambda: nc.vector.memset(onesb[:, :], 1.0), w=[onesb])
    ld("pool", ltb, ltb[:, :], I["c_lt"].ap())
    ld("pool", maskb, maskb[:, :], I["c_mask"].ap())
    ld("sp", slotc, slotc[:, :], I["c_slot"].ap())
    ld("sp", retc, retc[:, :, :], I["c_ret"].ap())
    ld("sp", valid0, valid0[:, :], I["c_valid0"].ap())
    ld("sp", rbias, rbias[:, :, :], I["router_b"].ap())
    for l in range(2):
        ld("sp", wr[l], wr[l][:, :, :], I["router_w"].ap()[l].rearrange("(c p) n -> p c n", p=128))

    ARENA_W = (nc.sbuf_bytes_remaining - 1024) // 4
    arena = nc.alloc_sbuf_tensor("arena", [128, ARENA_W], I32)
    ar = {"off": 0, "n": 0}
    DSZ = {F32: 4, I32: 4, BF16: 2}

    def AT(name, shape, dtype):
        n = int(np.prod(shape[1:]))
        words = (n * DSZ[dtype] + 3) // 4
        words = (words + 7) // 8 * 8
        off = ar["off"]
        ar["off"] += words
        assert ar["off"] <= ARENA_W, f"arena overflow at {name}: {ar['off']} > {ARENA_W}"
        v = arena[0:shape[0], off:off + words]
        if dtype != I32:
            v = v.bitcast(dtype)
        v = v[:, 0:n]
        if len(shape) == 3:
            v = v.rearrange("p (a b) -> p a b", a=shape[1])
        elif len(shape) == 4:
            v = v.rearrange("p (a b c) -> p a b c", a=shape[1], b=shape[2])
        t = Tl.__new__(Tl)
        t.t = v
        t.b = Buf()
        return t

    def phase(name):
        toks = [(kb.sem[e], kb.cnt[e], e) for e in ("pe", "act", "dve", "pool") if kb.cnt[e] > 0]
        for q in ("sp", "pool"):
            toks += [(kb.dpool[q][i], kb.dcnt[q][i], "dma") for i in range(len(kb.dpool[q])) if kb.dcnt[q][i] > 0]
        for e in ("pe", "act", "dve", "pool", "sp"):
            for tk in toks:
                if tk[2] == e and e != "pe":
                    kb.eng[e].wait_ge(tk[0], tk[1])
                    kb.seen[e][id(tk[0])] = tk[1]
                elif tk[2] != e:
                    kb._wait(e, tk)
        ar["off"] = 0

    class Rot:
        def __init__(self, name, shape, dtype, n, alloc=None):
            alloc = alloc or AT
            self.l = [alloc(f"{name}{i}", shape, dtype) for i in range(n)]
            self.i = 0

        def get(self):
            t = self.l[self.i % len(self.l)]
            self.i += 1
            return t

    cur = {}

    def rstd_of(src_tl, src_ap, n):
        s = cur["st"].get()
        junk = cur["junk"]
        V(lambda: nc.vector.scalar_tensor_tensor(out=junk[:, 0:n], in0=src_ap, scalar=1.0, in1=src_ap,
                                                 op0=ALU.mult, op1=ALU.mult, accum_out=s[:, 0:1]), r=[src_tl], w=[junk, s])
        V(lambda: nc.vector.tensor_scalar(out=s[:, 1:2], in0=s[:, 0:1], scalar1=1.0 / n, scalar2=EPS, op0=ALU.mult, op1=ALU.add), r=[s], w=[s])
        A(lambda: nc.scalar.activation(out=s[:, 2:3], in_=s[:, 1:2], func=AF.Ln), r=[s], w=[s])
        A(lambda: nc.scalar.activation(out=s[:, 3:4], in_=s[:, 2:3], func=AF.Exp, scale=-0.5), r=[s], w=[s])
        return s

    def common_bufs():
        cur["st"] = Rot("st", [128, 8], F32, 8)
        cur["junk"] = AT("junk", [128, D], BF16)

    def dbg_out(name, tl, ap, shape, dtype=F32):
        d = nc.dram_tensor("dbg_" + name, shape, dtype, kind="ExternalOutput")
        b = Buf()
        DMA("sp", lambda e: e.dma_start(out=d.ap(), in_=ap), r=[tl], w=[b])
        dbg_d[name] = b

    def dbg_dram(name, src, shape, dtype, deps):
        d = nc.dram_tensor("dbg_" + name, shape, dtype, kind="ExternalOutput")
        b = Buf()
        DMA("sp", lambda e: e.dma_start(out=d.ap(), in_=src), r=deps, w=[b])
        dbg_d[name] = b

    def finish(extra):
        kb.finish(list(extra) + list(dbg_d.values()))
        print("instructions", kb.n_inst, "waits", kb.n_wait, "arena words", ARENA_W)
        return nc

    def idle(n):
        for _ in range(n):
            yield "idle"

    def pipeline(make_gens, depth, skew):
        it = iter(make_gens)
        active = []
        done = False
        while True:
            while not done and len(active) < depth and (not active or active[-1][1] >= skew):
                mk = next(it, None)
                if mk is None:
                    done = True
                    break
                active.append([mk(), 0])
            if not active:
                break
            for a in list(active):
                try:
                    next(a[0])
                    a[1] += 1
                except StopIteration:
                    active.remove(a)

    def route_bufs(gain_tl, gain_ap, n=2):
        return dict(xnf=Rot("xnf", [128, D], F32, n), xnT=Rot("xnT", [128, 8, 128], F32, n),
                    xs=[AT(f"xs{i}", [128, 2, XW], I32) for i in range(n)], rt=Rot("rt", [128, 160], F32, 2),
                    desti=Rot("desti", [128, 2], I32, 4), Ab=Rot("Ab", [128, NE], BF16, n), gain_tl=gain_tl, gain_ap=gain_ap)

    def route_and_scatter(l, t, h_tl, h_ap, psT0, psT1, psS, rb, bgslack=0):
        xnf, xnT, Ab = rb["xnf"].get(), rb["xnT"].get(), rb["Ab"].get()
        s = rstd_of(h_tl, h_ap, D)
        V(lambda: nc.vector.scalar_tensor_tensor(out=xnf[:, :], in0=h_ap, scalar=s[:, 3:4], in1=rb["gain_ap"],
                                                 op0=ALU.mult, op1=ALU.mult), r=[h_tl, s, rb["gain_tl"]], w=[xnf])
        x2 = rb["xs"][t % len(rb["xs"])]
        xb = x2[:, :, :].bitcast(BF16)
        A(lambda: nc.scalar.activation(out=xb[:, 0, 0:D], in_=xnf[:, :], func=AF.Copy), r=[xnf], w=[x2])
        A(lambda: nc.scalar.activation(out=xb[:, 1, 0:D], in_=xnf[:, :], func=AF.Copy), r=[xnf], w=[x2])
        yield
        yield from idle(3 * bgslack)
        for half, ps in ((0, psT0), (1, psT1)):
            for c in range(4 * half, 4 * half + 4):
                PE(lambda c=c, ps=ps: nc.tensor.transpose(out=ps[:, (c % 4) * 128:(c % 4 + 1) * 128], in_=xnf[:, c * 128:(c + 1) * 128],
                                                          identity=identf[:, :]), r=[xnf, identf], w=[ps], inc=(c % 4 == 3))
            if half == 0:
                A(lambda: nc.scalar.activation(out=xnT[:, 0:4, :], in_=psT0[:, :].rearrange("p (c k) -> p c k", c=4), func=AF.Copy), r=[psT0], w=[xnT])
            else:
                V(lambda: nc.vector.tensor_copy(out=xnT[:, 4:8, :], in_=psT1[:, :].rearrange("p (c k) -> p c k", c=4)), r=[psT1], w=[xnT])
        yield
        yield from idle(bgslack)
        for c in range(8):
            PE(lambda c=c: nc.tensor.matmul(psS[:, 0:36], lhsT=xnT[:, c, :], rhs=wr[l][:, c, :], start=(c == 0), stop=(c == 7)),
               r=[xnT, wr[l]], w=[psS], inc=(c == 7))
        r = rb["rt"].get()
        yield
        V(lambda: nc.vector.tensor_tensor(out=r[:, 0:36], in0=psS[:, 0:36], in1=rbias[:, l, :], op=ALU.add), r=[psS, rbias], w=[r])
        V(lambda: nc.vector.tensor_reduce(out=r[:, 40:41], in_=r[:, 0:4], axis=AX.X, op=ALU.max), r=[r], w=[r])
        V(lambda: nc.vector.tensor_scalar(out=r[:, 41:42], in0=r[:, 40:41], scalar1=-1.0, scalar2=None, op0=ALU.mult), r=[r], w=[r])
        A(lambda: nc.scalar.activation(out=r[:, 36:40], in_=r[:, 0:4], func=AF.Exp, bias=r[:, 41:42], scale=1.0, accum_out=r[:, 42:43]), r=[r], w=[r])
        V(lambda: nc.vector.reciprocal(out=r[:, 43:44], in_=r[:, 42:43]), r=[r], w=[r])
        V(lambda: nc.vector.tensor_scalar(out=r[:, 44:48], in0=r[:, 0:4], scalar1=r[:, 40:41], scalar2=None, op0=ALU.is_equal), r=[r], w=[r])
        V(lambda: nc.vector.tensor_scalar(out=r[:, 44:48], in0=r[:, 44:48], scalar1=1.0, scalar2=1e30, op0=ALU.subtract, op1=ALU.mult), r=[r], w=[r])
        V(lambda: nc.vector.tensor_tensor(out=r[:, 48:80].rearrange("p (g e) -> p g e", g=4), in0=r[:, 4:36].rearrange("p (g e) -> p g e", g=4),
                                          in1=r[:, 44:48].unsqueeze(2).broadcast_to([128, 4, 8]), op=ALU.add), r=[r], w=[r])
        V(lambda: nc.vector.max(out=r[:, 80:88], in_=r[:, 48:80]), r=[r], w=[r])
        V(lambda: nc.vector.tensor_tensor(out=r[:, 88:89], in0=r[:, 81:82], in1=r[:, 80:81], op=ALU.subtract), r=[r], w=[r])
        A(lambda: nc.scalar.activation(out=r[:, 89:90], in_=r[:, 88:89], func=AF.Exp), r=[r], w=[r])
        V(lambda: nc.vector.tensor_scalar(out=r[:, 90:91], in0=r[:, 89:90], scalar1=1.0, scalar2=None, op0=ALU.add), r=[r], w=[r])
        V(lambda: nc.vector.reciprocal(out=r[:, 91:92], in_=r[:, 90:91]), r=[r], w=[r])
        V(lambda: nc.vector.tensor_tensor(out=r[:, 92:93], in0=r[:, 91:92], in1=r[:, 43:44], op=ALU.mult), r=[r], w=[r])
        V(lambda: nc.vector.tensor_tensor(out=r[:, 93:94], in0=r[:, 92:93], in1=r[:, 89:90], op=ALU.mult), r=[r], w=[r])
        V(lambda: nc.vector.tensor_scalar(out=r[:, 96:128], in0=r[:, 48:80], scalar1=r[:, 80:81], scalar2=None, op0=ALU.is_equal), r=[r], w=[r])
        V(lambda: nc.vector.tensor_scalar(out=r[:, 128:160], in0=r[:, 48:80], scalar1=r[:, 81:82], scalar2=None, op0=ALU.is_equal), r=[r], w=[r])
        yield
        yield from idle(5 * bgslack)
        if t == 0:
            V(lambda: nc.vector.scalar_tensor_tensor(out=Ab[:, :], in0=r[:, 96:128], scalar=valid0[:, 0:1], in1=r[:, 128:160],
                                                     op0=ALU.mult, op1=ALU.add), r=[r, valid0], w=[Ab])
            V(lambda: nc.vector.tensor_scalar(out=Ab[:, :], in0=Ab[:, :], scalar1=valid0[:, 0:1], scalar2=1.0, op0=ALU.mult, op1=ALU.min), r=[Ab, valid0], w=[Ab])
        else:
            V(lambda: nc.vector.tensor_tensor(out=Ab[:, :], in0=r[:, 96:128], in1=r[:, 128:160], op=ALU.add), r=[r], w=[Ab])
        PE(lambda: nc.tensor.matmul(psS[:, 64:96], lhsT=ltb[:, :], rhs=Ab[:, :], start=True, stop=True), r=[ltb, Ab], w=[psS], inc=False)
        PE(lambda: nc.tensor.matmul(psS[:, 128:160], lhsT=onesb[:, :], rhs=Ab[:, :], start=True, stop=True), r=[onesb, Ab], w=[psS])
        V(lambda: nc.vector.tensor_tensor(out=r[:, 48:80], in0=psS[:, 64:96], in1=base[:, :], op=ALU.add), r=[psS, base, r], w=[r])
        V(lambda: nc.vector.tensor_tensor(out=base[:, :], in0=psS[:, 128:160], in1=base[:, :], op=ALU.add), r=[psS], w=[base])
        V(lambda: nc.vector.tensor_tensor(out=r[:, 0:32], in0=r[:, 48:80], in1=slotc[:, 32:64], op=ALU.is_ge), r=[r, slotc], w=[r])
        V(lambda: nc.vector.scalar_tensor_tensor(out=r[:, 48:80], in0=r[:, 0:32], scalar=1e7, in1=r[:, 48:80], op0=ALU.mult, op1=ALU.add), r=[r], w=[r])
        V(lambda: nc.vector.scalar_tensor_tensor(out=r[:, 0:32], in0=r[:, 96:128], scalar=1.0, in1=r[:, 48:80],
                                                 op0=ALU.mult, op1=ALU.mult, accum_out=r[:, 36:37]), r=[r], w=[r])
        V(lambda: nc.vector.scalar_tensor_tensor(out=r[:, 0:32], in0=r[:, 128:160], scalar=1.0, in1=r[:, 48:80],
                                                 op0=ALU.mult, op1=ALU.mult, accum_out=r[:, 37:38]), r=[r], w=[r])
        if t == 0:
            V(lambda: nc.vector.tensor_scalar(out=r[:, 36:38], in0=r[:, 36:38], scalar1=valid0[:, 1:2], scalar2=None, op0=ALU.add), r=[r, valid0], w=[r])
        V(lambda: nc.vector.tensor_scalar(out=r[:, 36:38], in0=r[:, 36:38], scalar1=3e7, scalar2=None, op0=ALU.min), r=[r], w=[r])
        yield
        di = rb["desti"].get()
        V(lambda: nc.vector.tensor_copy(out=di[:, :], in_=r[:, 36:38]), r=[r], w=[di])
        for k in range(2):
            V(lambda k=k: nc.vector.tensor_copy(out=x2[:, k, 512:513], in_=retc[:, t, k:k + 1]), r=[retc], w=[x2])
            V(lambda k=k: nc.vector.tensor_copy(out=x2[:, k, 513:514].bitcast(F32), in_=r[:, 92 + k:93 + k]), r=[r], w=[x2])
        for k in range(2):
            DMA("pool", lambda e, k=k: e.indirect_dma_start(out=xs_d[l][:, :], out_offset=bass.IndirectOffsetOnAxis(ap=di[:, k:k + 1], axis=0),
                                                             in_=x2[:, k, :], in_offset=None, bounds_check=reg_xs, oob_is_err=False),
                r=[x2, di, b_xsi[l]], s=[b_xs[l]])

    def pass1():
        common_bufs()
        xinit = AT("xinit", [128, NSLOT // 128, 2], I32)
        ld("sp", xinit, xinit[:, :, :], I["c_xinit"].ap())
        NPP = NSLOT // 128
        for l in range(2):
            for j in range(8):
                DMA("sp", lambda e, l=l, j=j: e.dma_start(
                    out=xs_d[l].ap().rearrange("(p n) w -> p n w", p=128)[:, j * (NPP // 8):(j + 1) * (NPP // 8), 512:514],
                    in_=xinit[:, j * (NPP // 8):(j + 1) * (NPP // 8), :]), r=[xinit], s=[b_xsi[l]])
        gt = AT("g1", [128, 3, D], F32)
        ld("sp", gt, gt[:, 0:2, :], I["gains"].ap()[:, G_ANORM:G_ASCALE + 1, :])
        ld("sp", gt, gt[:, 2, :], I["gains"].ap()[:, G_FFN0, :])
        poolc = AT("poolc", [128, 3, 4, 128], BF16)
        ld("pool", poolc, poolc[:, :, :, :], I["c_pool"].ap().rearrange("a g i o -> i a g o"))
        aw0 = AT("aw0", [128, 4, 2, 256], F32)
        for g in range(4):
            DMA("sp", lambda e, g=g: e.dma_start(out=aw0[:, g, :, :], in_=I["a_w"].ap()[g].rearrange("(kc p) f -> p kc f", p=128)), s=[aw0])
        aw = AT("aw", [128, 4, 2, 256], BF16)
        for g in range(4):
            V(lambda g=g: nc.vector.tensor_tensor(out=aw[:, g, :, :], in0=aw0[:, g, :, :],
                                                  in1=gt[:, 1, g * 256:(g + 1) * 256].unsqueeze(1).broadcast_to([128, 2, 256]), op=ALU.mult),
              r=[aw0, gt], w=[aw])
        V(lambda: nc.vector.tensor_copy(out=base[:, :], in_=slotc[:, 0:32]), r=[slotc], w=[base])
        h0 = [AT(f"h0_{i}", [128, D], F32) for i in range(3)]
        hnb = [AT(f"hnb{i}", [128, D], BF16) for i in range(2)]
        plT = AT("plT", [128, 8, 128], BF16)
        tmpf = AT("tmpf", [128, D], F32)
        rb = route_bufs(gt, gt[:, 2, :])

        hnb3 = hnb + [AT("hnb2", [128, D], BF16)]
        plT2 = [plT, AT("plT_b", [128, 8, 128], BF16)]
        tmpf2 = [tmpf, AT("tmpf_b", [128, D], F32)]

        def p1_tile(t):
            hb = h0[t % 3]
            if t == 0:
                V(lambda: nc.vector.memset(hb[:, :], 0.0), w=[hb])
                DMA("sp", lambda e: e.dma_start(out=hb[112:128, :], in_=I["meta_tokens"].ap()), w=[hb])
            else:
                ld("sp", hb, hb[:, :], I["x"].ap()[(t - 1) * 128:t * 128, :])
            yield
            s = rstd_of(hb, hb[:, :], D)
            hn = hnb3[t % 3]
            hp = hnb3[(t - 1) % 3]
            pl = plT2[t % 2]
            tm = tmpf2[t % 2]
            V(lambda: nc.vector.scalar_tensor_tensor(out=hn[:, :], in0=hb[:, :], scalar=s[:, 3:4], in1=gt[:, 0, :],
                                                     op0=ALU.mult, op1=ALU.mult), r=[hb, s, gt], w=[hn])
            yield
            for cc in range(8):
                g = cc // 2
                ps = PS[0] if cc < 4 else PS[1]
                o = ps[:, (cc % 4) * 128:(cc % 4 + 1) * 128]
                if t == 0:
                    PE(lambda cc=cc, o=o, g=g: nc.tensor.matmul(o, lhsT=hn[:, cc * 128:(cc + 1) * 128], rhs=poolc[:, 2, g, :], start=True, stop=True),
                       r=[hn, poolc], w=[ps], inc=(cc % 4 == 3))
                else:
                    PE(lambda cc=cc, o=o, g=g: nc.tensor.matmul(o, lhsT=hn[:, cc * 128:(cc + 1) * 128], rhs=poolc[:, 0, g, :], start=True, stop=False),
                       r=[hn, poolc], w=[ps], inc=False)
                    PE(lambda cc=cc, o=o, g=g: nc.tensor.matmul(o, lhsT=hp[:, cc * 128:(cc + 1) * 128], rhs=poolc[:, 1, g, :], start=False, stop=True),
                       r=[hp, poolc], w=[ps], inc=(cc % 4 == 3))
            A(lambda: nc.scalar.activation(out=pl[:, 0:4, :], in_=PS[0][:, :].rearrange("p (c k) -> p c k", c=4), func=AF.Copy), r=[PS[0]], w=[pl])
            A(lambda: nc.scalar.activation(out=pl[:, 4:8, :], in_=PS[1][:, :].rearrange("p (c k) -> p c k", c=4), func=AF.Copy), r=[PS[1]], w=[pl])
            yield
            for g in range(4):
                ps = PS[2] if g < 2 else PS[3]
                for kc in range(2):
                    PE(lambda g=g, kc=kc, ps=ps: nc.tensor.matmul(ps[:, (g % 2) * 256:(g % 2 + 1) * 256], lhsT=pl[:, 2 * g + kc, :], rhs=aw[:, g, kc, :],
                                                                  start=(kc == 0), stop=(kc == 1)), r=[pl, aw], w=[ps], inc=(g % 2 == 1 and kc == 1))
            for hf in range(2):
                V(lambda hf=hf: nc.vector.tensor_tensor(out=hb[:, hf * 512:(hf + 1) * 512], in0=PS[2 + hf][:, :], in1=hb[:, hf * 512:(hf + 1) * 512],
                                                        op=ALU.add), r=[PS[2 + hf]], w=[hb])
            yield
            DMA("pool", lambda e: e.dma_start(out=h_d[t * 128:(t + 1) * 128, :], in_=hb[:, :]), r=[hb], w=[b_h[t]])
            yield from route_and_scatter(0, t, hb, hb[:, :], PS[4], PS[5], PS[6], rb)

        pipeline([(lambda t=t: p1_tile(t)) for t in range(NT)], depth=3, skew=3)

    def expert_loop(l):
        NWB = 4
        NXB = 4 * ST
        wgt = [AT(f"wg{i}", [128, 2048], BF16) for i in range(NWB)]
        wut = [AT(f"wu{i}", [128, 2048], BF16) for i in range(NWB)]
        wdt = [AT(f"wd{i}", [128, 2048], BF16) for i in range(NWB)]
        xt = [AT(f"xt{i}", [128, XW], I32) for i in range(NXB)]
        xeT = [AT(f"xeT{i}", [128, 8, CAP], BF16) for i in range(2)]
        sgt = [AT(f"sg{i}", [128, CAP], F32) for i in range(2)]
        cnt_t = AT("cnt_dbg", [128, NE], F32)
        hact = [AT(f"hact{i}", [128, 2, CAP], BF16) for i in range(2)]
        ysb = Rot("ysb", [128, D], F32, 3)

        def ex_load(e):
            i = e % NWB
            ld("pool", wgt[i], wgt[i][:, :], I["w_gate"].ap()[l, e].rearrange("(p c) f -> p (c f)", c=8))
            ld("pool", wut[i], wut[i][:, :], I["w_up"].ap()[l, e].rearrange("(p c) f -> p (c f)", c=8))
            ld("pool", wdt[i], wdt[i][:, :], I["w_down"].ap()[l, e].rearrange("(p c) d -> p (c d)", c=2))
            for s_ in range(ST):
                x = xt[(e * ST + s_) % NXB]
                r0 = (e * ST + s_) * 128
                ld("sp", x, x[:, :], xs_d[l][r0:r0 + 128, :], r=[b_xs[l]])

        def ex_compute(e):
            if e + 2 < NE:
                ex_load(e + 2)
            i = e % NWB
            xe = xeT[e % 2]
            ha = hact[e % 2]
            for s_ in range(ST):
                x = xt[(e * ST + s_) % NXB]
                xv = x[:, 0:512].bitcast(BF16).rearrange("s (p c) -> s c p", c=8)
                ps = PS[s_ % 2]
                pv = ps[:, :].bitcast(BF16).rearrange("p (c k) -> p c k", c=8)
                for c in range(8):
                    PE(lambda c=c, pv=pv, xv=xv: nc.tensor.transpose(out=pv[:, c, :], in_=xv[:, c, :], identity=ident[:, :]),
                       r=[x, ident], w=[ps], inc=(c == 7))
                if s_ % 2 == 0:
                    A(lambda pv=pv, s_=s_: nc.scalar.activation(out=xe[:, :, s_ * 128:(s_ + 1) * 128], in_=pv, func=AF.Copy), r=[ps], w=[xe])
                else:
                    V(lambda pv=pv, s_=s_: nc.vector.tensor_copy(out=xe[:, :, s_ * 128:(s_ + 1) * 128], in_=pv), r=[ps], w=[xe])
                yield
            wgv = wgt[i][:, :].rearrange("p (c f two) -> p c two f", c=8, two=2)
            wuv = wut[i][:, :].rearrange("p (c f two) -> p c two f", c=8, two=2)
            wdv = wdt[i][:, :].rearrange("p (c d) -> p c d", c=2)
            for fc in range(2):
                for wv, wtl, ps in ((wgv, wgt[i], PS[2 + fc]), (wuv, wut[i], PS[4 + fc])):
                    for c in range(8):
                        PE(lambda c=c, wv=wv, ps=ps, fc=fc: nc.tensor.matmul(ps[:, 0:CAP], lhsT=wv[:, c, fc, :], rhs=xe[:, c, :], start=(c == 0), stop=(c == 7)),
                           r=[wtl, xe], w=[ps], inc=(c == 7))
                A(lambda fc=fc: nc.scalar.activation(out=sgt[fc][:, :], in_=PS[2 + fc][:, 0:CAP], func=AF.Silu), r=[PS[2 + fc]], w=[sgt[fc]])
                V(lambda fc=fc: nc.vector.tensor_tensor(out=ha[:, fc, :], in0=sgt[fc][:, :], in1=PS[4 + fc][:, 0:CAP], op=ALU.mult),
                  r=[sgt[fc], PS[4 + fc]], w=[ha])
                yield
            for s_ in range(ST):
                x = xt[(e * ST + s_) % NXB]
                for hf in range(2):
                    ps = PS[6 + hf]
                    for fc in range(2):
                        PE(lambda fc=fc, hf=hf, ps=ps, s_=s_: nc.tensor.matmul(ps[:, :], lhsT=ha[:, fc, s_ * 128:(s_ + 1) * 128], rhs=wdv[:, fc, hf * 512:(hf + 1) * 512],
                                                                              start=(fc == 0), stop=(fc == 1)), r=[ha, wdt[i]], w=[ps], inc=(fc == 1))
                y = ysb.get()
                gate_ap = x[:, 513:514].bitcast(F32)
                A(lambda y=y, gate_ap=gate_ap: nc.scalar.activation(out=y[:, 0:512], in_=PS[6][:, :], func=AF.Copy, scale=gate_ap), r=[PS[6], x], w=[y])
                V(lambda y=y, gate_ap=gate_ap: nc.vector.tensor_scalar(out=y[:, 512:1024], in0=PS[7][:, :], scalar1=gate_ap, scalar2=None, op0=ALU.mult), r=[PS[7], x], w=[y])
                DMA("pool", lambda e_, y=y, x=x: e_.indirect_dma_start(out=yk_d[l][:, :], out_offset=bass.IndirectOffsetOnAxis(ap=x[:, 512:513], axis=0),
                                                                        in_=y[:, :], in_offset=None, bounds_check=reg_yk, oob_is_err=False),
                    r=[y, x], s=[b_yk[l]])
                yield

        ex_load(0)
        ex_load(1)
        pipeline([(lambda e=e: ex_compute(e)) for e in range(NE)], depth=2, skew=ST)

    def pass2a():
        common_bufs()
        gk = AT("gk", [128, D], F32)
        ld("sp", gk, gk[:, :], I["gains"].ap()[:, G_KV, :])
        gl = AT("gl", [128, 256], F32)
        ld("sp", gl, gl[:, :], I["kvl_gain"].ap())
        wdkv = AT("wdkv", [128, 8, 384], BF16)
        wsrc = I["w_dkv"].ap().rearrange("(c p) n -> p c n", p=128)
        DMA("pool", lambda e: e.dma_start(out=wdkv[:, :, 0:320], in_=wsrc), w=[wdkv])
        DMA("pool", lambda e: e.dma_start(out=wdkv[:, :, 320:352], in_=wsrc[:, :, 288:320]), s=[wdkv])
        DMA("pool", lambda e: e.dma_start(out=wdkv[:, :, 352:384], in_=wsrc[:, :, 256:288]), s=[wdkv])
        wuk = AT("wuk", [128, 2, D], BF16)
        ld("pool", wuk, wuk[:, :, :], I["w_uk"].ap().rearrange("(rc p) n -> p rc n", p=128))
        wuv = AT("wuv", [128, 2, D], BF16)
        ld("pool", wuv, wuv[:, :, :], I["w_uv"].ap().rearrange("(rc p) n -> p rc n", p=128))
        hA = [AT(f"hA{i}", [128, D], F32) for i in range(3)]
        ykb = [AT(f"ykb{i}", [128, 2, D], F32) for i in range(3)]
        ropec = [AT(f"ropec{i}", [64, 2, 128], F32) for i in range(3)]
        hnk_l = [AT(f"hnk{i}", [128, D], BF16) for i in range(2)]
        hnT_l = [AT(f"hnT{i}", [128, 8, 128], BF16) for i in range(2)]
        clat_l = [AT(f"clat{i}", [128, 256], F32) for i in range(2)]
        ckvn_l = [AT(f"ckvn{i}", [128, 256], BF16) for i in range(2)]
        ckT_l = [AT(f"ckT{i}", [128, 2, 128], BF16) for i in range(2)]
        kT = [AT(f"kT{i}", [128, 8, 128], BF16) for i in range(2)]
        vt = [AT(f"vt{i}", [128, 8, 130], BF16) for i in range(3)]
        krT = [AT(f"krT{i}", [64, 128], BF16) for i in range(2)]
        rtmp_l = [AT(f"rtmp{i}", [64, 2, 128], F32) for i in range(2)]
        for i in range(3):
            V(lambda i=i: nc.vector.memset(vt[i][:, :, 128:130], 1.0), w=[vt[i]])

        def tile(t):
            h = hA[t % 3]
            y = ykb[t % 3]
            hnk, hnT, clat, ckvn, ckT, rtmp = hnk_l[t % 2], hnT_l[t % 2], clat_l[t % 2], ckvn_l[t % 2], ckT_l[t % 2], rtmp_l[t % 2]
            ld("sp", hA[t % 3], hA[t % 3][:, :], h_d[t * 128:(t + 1) * 128, :], r=[b_h[t]])
            ld("sp", ykb[t % 3], ykb[t % 3][:, :, :], yk_d[0][t * 256:(t + 1) * 256, :].rearrange("(p k) d -> p k d", k=2), r=[b_yk[0]])
            ld("sp", ropec[t % 3], ropec[t % 3][:, :, :], I["c_rope"].ap()[:, :, t * 128:(t + 1) * 128].rearrange("a j k -> j a k"))
            yield
            V(lambda: nc.vector.tensor_tensor(out=h[:, :], in0=h[:, :], in1=y[:, 0, :], op=ALU.add), r=[y], w=[h])
            G(lambda: nc.gpsimd.tensor_tensor(out=h[:, :], in0=h[:, :], in1=y[:, 1, :], op=ALU.add), r=[y], w=[h])
            if t == 0:
                V(lambda: nc.vector.memset(h[0:112, :], 0.0), w=[h])
            DMA("pool", lambda e: e.dma_start(out=h_d[t * 128:(t + 1) * 128, :], in_=h[:, :]), r=[h], w=[b_h[t]])
            s = rstd_of(h, h[:, :], D)
            V(lambda: nc.vector.scalar_tensor_tensor(out=hnk[:, :], in0=h[:, :], scalar=s[:, 3:4], in1=gk[:, :], op0=ALU.mult, op1=ALU.mult),
              r=[h, s, gk], w=[hnk])
            yield
            pv = PS[0][:, :].bitcast(BF16).rearrange("p (c k) -> p c k", c=8)
            for c in range(8):
                PE(lambda c=c: nc.tensor.transpose(out=pv[:, c, :], in_=hnk[:, c * 128:(c + 1) * 128], identity=ident[:, :]),
                   r=[hnk, ident], w=[PS[0]], inc=(c == 7))
            A(lambda: nc.scalar.activation(out=hnT[:, :, :], in_=pv, func=AF.Copy), r=[PS[0]], w=[hnT])
            for c in range(8):
                PE(lambda c=c: nc.tensor.matmul(PS[1][:, 0:256], lhsT=hnT[:, c, :], rhs=wdkv[:, c, 0:256], start=(c == 0), stop=(c == 7)),
                   r=[hnT, wdkv], w=[PS[1]], inc=(c == 7))
            yield
            for j in range(2):
                for c in range(8):
                    PE(lambda c=c, j=j: nc.tensor.matmul(PS[2][0:64, j * 128:(j + 1) * 128], lhsT=wdkv[:, c, 256 + 64 * j:320 + 64 * j], rhs=hnT[:, c, :],
                                                         start=(c == 0), stop=(c == 7)), r=[hnT, wdkv], w=[PS[2]], inc=(c == 7))
            rc_ = ropec[t % 3]
            V(lambda: nc.vector.tensor_tensor(out=rtmp[:, :, :], in0=PS[2][0:64, 0:256].rearrange("p (a k) -> p a k", a=2), in1=rc_[:, :, :], op=ALU.mult),
              r=[PS[2], rc_], w=[rtmp])
            kr = krT[t % 2]
            V(lambda: nc.vector.tensor_tensor(out=kr[:, :], in0=rtmp[:, 0, :], in1=rtmp[:, 1, :], op=ALU.add), r=[rtmp], w=[kr])
            DMA("pool", lambda e: e.dma_start(out=kr_d.ap()[:, t, :], in_=kr[:, :]), r=[kr], s=[b_kv[t]])
            yield
            A(lambda: nc.scalar.activation(out=clat[:, :], in_=PS[1][:, 0:256], func=AF.Copy), r=[PS[1]], w=[clat])
            s2 = rstd_of(clat, clat[:, :], 256)
            V(lambda: nc.vector.scalar_tensor_tensor(out=ckvn[:, :], in0=clat[:, :], scalar=s2[:, 3:4], in1=gl[:, :], op0=ALU.mult, op1=ALU.mult),
              r=[clat, s2, gl], w=[ckvn])
            pv3 = PS[3][:, 0:128].bitcast(BF16).rearrange("p (c k) -> p c k", c=2)
            for c in range(2):
                PE(lambda c=c: nc.tensor.transpose(out=pv3[:, c, :], in_=ckvn[:, c * 128:(c + 1) * 128], identity=ident[:, :]),
                   r=[ckvn, ident], w=[PS[3]], inc=(c == 1))
            V(lambda: nc.vector.tensor_copy(out=ckT[:, :, :], in_=pv3), r=[PS[3]], w=[ckT])
            yield
            for hh in range(8):
                ps = PS[4 + hh // 4]
                for rc in range(2):
                    PE(lambda hh=hh, rc=rc, ps=ps: nc.tensor.matmul(ps[:, (hh % 4) * 128:(hh % 4 + 1) * 128], lhsT=wuk[:, rc, hh * 128:(hh + 1) * 128], rhs=ckT[:, rc, :],
                                                                    start=(rc == 0), stop=(rc == 1)), r=[wuk, ckT], w=[ps], inc=(hh % 4 == 3 and rc == 1))
            k_ = kT[t % 2]
            A(lambda: nc.scalar.activation(out=k_[:, 0:4, :], in_=PS[4][:, :].rearrange("p (c k) -> p c k", c=4), func=AF.Copy), r=[PS[4]], w=[k_])
            V(lambda: nc.vector.tensor_copy(out=k_[:, 4:8, :], in_=PS[5][:, :].rearrange("p (c k) -> p c k", c=4)), r=[PS[5]], w=[k_])
            DMA("pool", lambda e: e.dma_start(out=kt_d.ap()[:, :, t, :].rearrange("h p k -> p h k"), in_=k_[:, :, :]), r=[k_], s=[b_kv[t]])
            yield
            for hf in range(2):
                for rc in range(2):
                    PE(lambda hf=hf, rc=rc: nc.tensor.matmul(PS[6 + hf][:, :], lhsT=ckT[:, rc, :], rhs=wuv[:, rc, hf * 512:(hf + 1) * 512],
                                                             start=(rc == 0), stop=(rc == 1)), r=[ckT, wuv], w=[PS[6 + hf]], inc=(rc == 1))
            v_ = vt[t % 3]
            A(lambda: nc.scalar.activation(out=v_[:, 0:4, 0:128], in_=PS[6][:, :].rearrange("p (c k) -> p c k", c=4), func=AF.Copy), r=[PS[6]], w=[v_])
            V(lambda: nc.vector.tensor_copy(out=v_[:, 4:8, 0:128], in_=PS[7][:, :].rearrange("p (c k) -> p c k", c=4)), r=[PS[7]], w=[v_])
            if t == 0:
                V(lambda: nc.vector.memset(v_[0:112, :, :], 0.0), w=[v_])
            DMA("pool", lambda e: e.dma_start(out=vt_d.ap()[:, :, t, :].rearrange("h p n -> p h n"), in_=v_[:, :, :]), r=[v_], s=[b_kv[t]])
            if t == 0:
                V(lambda: nc.vector.memset(v_[:, :, 128:130], 1.0), w=[v_])

        pipeline([(lambda t=t: tile(t)) for t in range(NT)], depth=3, skew=2)

    def pass2b():
        common_bufs()
        gb = AT("gb", [128, 2, D], F32)
        ld("sp", gb, gb[:, 0, :], I["gains"].ap()[:, G_B, :])
        ld("sp", gb, gb[:, 1, :], I["gains"].ap()[:, G_FFN1, :])
        gq = AT("gq", [128, 384], F32)
        ld("sp", gq, gq[:, :], I["qn_gain"].ap())
        wdq = AT("wdq", [128, 8, 384], BF16)
        ld("pool", wdq, wdq[:, :, :], I["b_w_dq"].ap().rearrange("(c p) r -> p c r", p=128))
        wuqn = AT("wuqn", [128, 3, 8, 128], BF16)
        usrc = I["b_w_uq"].ap().rearrange("(rc p) h d -> p rc h d", p=128)
        wuqr = AT("wuqr", [128, 3, 8, 128], BF16)
        wuqs = AT("wuqs", [128, 3, 8, 128], BF16)
        for rc in range(3):
            DMA("pool", lambda e, rc=rc: e.dma_start(out=wuqn[:, rc, :, :], in_=usrc[:, rc, :, 0:128]), s=[wuqn])
            for dup in range(2):
                o = 64 * dup
                DMA("pool", lambda e, rc=rc, o=o: e.dma_start(out=wuqr[:, rc, :, o:o + 64], in_=usrc[:, rc, :, 128:192]), s=[wuqr])
                DMA("pool", lambda e, rc=rc, o=o: e.dma_start(out=wuqs[:, rc, :, o:o + 32], in_=usrc[:, rc, :, 160:192]), s=[wuqs])
                DMA("pool", lambda e, rc=rc, o=o: e.dma_start(out=wuqs[:, rc, :, o + 32:o + 64], in_=usrc[:, rc, :, 128:160]), s=[wuqs])
        wo = AT("wo", [128, 8, D], BF16)
        ld("pool", wo, wo[:, :, :], I["b_w_o"].ap().rearrange("(hh p) d -> p hh d", p=128))
        V(lambda: nc.vector.tensor_copy(out=base[:, :], in_=slotc[:, 0:32]), r=[slotc], w=[base])
        hqa = Rot("hqa", [128, D], F32, 2)
        hqe = Rot("hqe", [128, D], F32, 2)
        hnq = AT("hnq", [128, D], BF16)
        hnT = AT("hnT", [128, 8, 128], BF16)
        cq = AT("cq", [128, 384], F32)
        cqn = AT("cqn", [128, 384], BF16)
        cqT = [AT(f"cqT{i}", [128, 3, QC * 128], BF16) for i in range(2)]
        qT = [AT(f"qT{i}", [128, 8, QC * 128], BF16) for i in range(2)]
        qrT = [AT(f"qrT{i}", [128, 8, QC * 128], BF16) for i in range(2)]
        ropeq = AT("ropeq", [128, 2, QC * 128], F32)
        rq = AT("rq", [128, 2, QC * 128], F32)
        krc = AT("krc", [128, NT * 128], BF16)
        krc_b = [Buf() for _ in range(8)]
        Kh = [AT(f"Kh{i}", [128, NT * 128], BF16) for i in range(2)]
        Vh = [AT(f"Vh{i}", [128, NT, 130], BF16) for i in range(2)]
        pT = Rot("pT", [128, QC * 128], BF16, 6)
        rec = Rot("rec", [128, 4], F32, 2)
        osb = [AT(f"osb{i}", [128, QC, D], BF16) for i in range(2)]
        oT = AT("oT", [128, 8, 128], BF16)
        rb = route_bufs(gb, gb[:, 1, :], n=1)
        print("p2b arena words used", ar["off"], "of", ARENA_W)

        def kv_load(c, hh):
            nk = 4 * c + 5
            i = (c * 8 + hh) % 2
            deps = [b_kv[t] for t in range(nk)]
            ld("sp", Kh[i], Kh[i][:, 0:nk * 128], kt_d.ap()[hh, :, 0:nk, :].rearrange("p t k -> p (t k)"), r=deps)
            ld("sp", Vh[i], Vh[i][:, 0:nk, :], vt_d.ap()[hh, :, 0:nk, :], r=deps)

        def qpath(c):
            cp = c % 2
            t0 = 4 * c + 1
            gid0 = t0 * 128
            ta, tb = (0, 5) if c == 0 else (4 * c + 1, 4 * c + 5)
            for dup in range(2):
                DMA("sp", lambda e, dup=dup: e.dma_start(out=krc[64 * dup:64 * dup + 64, ta * 128:tb * 128],
                                                         in_=kr_d.ap()[:, ta:tb, :].rearrange("j t k -> j (t k)")),
                    r=[b_kv[t] for t in range(ta, tb)], s=[krc_b[c]])
                DMA("sp", lambda e, dup=dup: e.dma_start(out=ropeq[64 * dup:64 * dup + 64, :, :],
                                                         in_=I["c_rope"].ap()[:, :, gid0:gid0 + QC * 128].rearrange("a j k -> j a k")), s=[ropeq])
            for j in range(QC):
                t = t0 + j
                h = hqa.get()
                ld("sp", h, h[:, :], h_d[t * 128:(t + 1) * 128, :], r=[b_h[t]])
                s = rstd_of(h, h[:, :], D)
                V(lambda h=h, s=s: nc.vector.scalar_tensor_tensor(out=hnq[:, :], in0=h[:, :], scalar=s[:, 3:4], in1=gb[:, 0, :], op0=ALU.mult, op1=ALU.mult),
                  r=[h, s, gb], w=[hnq])
                yield
                yield from idle(5)
                pv = PS[6][:, :].bitcast(BF16).rearrange("p (c k) -> p c k", c=8)
                for cc in range(8):
                    PE(lambda cc=cc: nc.tensor.transpose(out=pv[:, cc, :], in_=hnq[:, cc * 128:(cc + 1) * 128], identity=ident[:, :]),
                       r=[hnq, ident], w=[PS[6]], inc=(cc == 7))
                A(lambda: nc.scalar.activation(out=hnT[:, :, :], in_=pv, func=AF.Copy), r=[PS[6]], w=[hnT])
                yield
                yield from idle(2)
                for cc in range(8):
                    PE(lambda cc=cc: nc.tensor.matmul(PS[7][:, 0:384], lhsT=hnT[:, cc, :], rhs=wdq[:, cc, :], start=(cc == 0), stop=(cc == 7)),
                       r=[hnT, wdq], w=[PS[7]], inc=(cc == 7))
                A(lambda: nc.scalar.activation(out=cq[:, :], in_=PS[7][:, 0:384], func=AF.Copy), r=[PS[7]], w=[cq])
                yield
                s2 = rstd_of(cq, cq[:, :], 384)
                V(lambda s2=s2: nc.vector.scalar_tensor_tensor(out=cqn[:, :], in0=cq[:, :], scalar=s2[:, 3:4], in1=gq[:, :], op0=ALU.mult, op1=ALU.mult),
                  r=[cq, s2, gq], w=[cqn])
                yield
                yield from idle(5)
                pv3 = PS[6][:, 0:192].bitcast(BF16).rearrange("p (c k) -> p c k", c=3)
                for rc in range(3):
                    PE(lambda rc=rc: nc.tensor.transpose(out=pv3[:, rc, :], in_=cqn[:, rc * 128:(rc + 1) * 128], identity=ident[:, :]),
                       r=[cqn, ident], w=[PS[6]], inc=(rc == 2))
                V(lambda j=j: nc.vector.tensor_copy(out=cqT[cp][:, :, j * 128:(j + 1) * 128], in_=pv3), r=[PS[6]], w=[cqT[cp]])
                yield
            for hh in range(8):
                for rc in range(3):
                    PE(lambda hh=hh, rc=rc: nc.tensor.matmul(PS[6][:, :], lhsT=wuqn[:, rc, hh, :], rhs=cqT[cp][:, rc, :], start=(rc == 0), stop=(rc == 2)),
                       r=[wuqn, cqT[cp]], w=[PS[6]], inc=(rc == 2))
                A(lambda hh=hh: nc.scalar.activation(out=qT[cp][:, hh, :], in_=PS[6][:, :], func=AF.Copy), r=[PS[6]], w=[qT[cp]])
                yield
                for rc in range(3):
                    PE(lambda hh=hh, rc=rc: nc.tensor.matmul(PS[7][:, :], lhsT=wuqr[:, rc, hh, :], rhs=cqT[cp][:, rc, :], start=(rc == 0), stop=(rc == 2)),
                       r=[wuqr, cqT[cp]], w=[PS[7]], inc=(rc == 2))
                V(lambda: nc.vector.tensor_tensor(out=rq[:, 0, :], in0=PS[7][:, :], in1=ropeq[:, 0, :], op=ALU.mult), r=[PS[7], ropeq], w=[rq])
                yield
                for rc in range(3):
                    PE(lambda hh=hh, rc=rc: nc.tensor.matmul(PS[6][:, :], lhsT=wuqs[:, rc, hh, :], rhs=cqT[cp][:, rc, :], start=(rc == 0), stop=(rc == 2)),
                       r=[wuqs, cqT[cp]], w=[PS[6]], inc=(rc == 2))
                V(lambda: nc.vector.tensor_tensor(out=rq[:, 1, :], in0=PS[6][:, :], in1=ropeq[:, 1, :], op=ALU.mult), r=[PS[6], ropeq], w=[rq])
                G(lambda hh=hh: nc.gpsimd.tensor_tensor(out=qrT[cp][:, hh, :], in0=rq[:, 0, :], in1=rq[:, 1, :], op=ALU.add), r=[rq], w=[qrT[cp]])
                yield

        def attention(c):
            cp = c % 2
            nk = 4 * c + 5
            t0 = 4 * c + 1
            steps = [(hh, kt) for hh in range(8) for kt in range(nk)]
            pend = {}

            def emit_S_pair(i0_, i1_):
                info = []
                pbase = 2 * ((i0_ // 2) % 2)
                for slot, idx in enumerate((i0_, i1_)):
                    hh, kt = steps[idx]
                    if kt == 0 and hh == 0:
                        kv_load(c, 0)
                        kv_load(c, 1)
                    K_ = Kh[(c * 8 + hh) % 2]
                    i0 = max(0, kt - t0)
                    col0 = i0 * 128
                    pss = PS[pbase + slot]
                    PE(lambda K_=K_, kt=kt, hh=hh, col0=col0, pss=pss: nc.tensor.matmul(pss[:, col0:512], lhsT=K_[:, kt * 128:(kt + 1) * 128],
                                                                                        rhs=qT[cp][:, hh, col0:512], start=True, stop=False),
                       r=[K_, qT[cp]], w=[pss], inc=False)
                    info.append((hh, kt, i0, col0, pss))
                for slot, (hh, kt, i0, col0, pss) in enumerate(info):
                    diag = kt >= t0
                    kseg = krc_b[0] if kt < 5 else krc_b[(kt - 1) // 4]
                    r0 = 64 * slot
                    PE(lambda kt=kt, hh=hh, col0=col0, pss=pss, r0=r0: nc.tensor.matmul(pss[:, col0:512], lhsT=krc[r0:r0 + 64, kt * 128:(kt + 1) * 128],
                                                                                        rhs=qrT[cp][r0:r0 + 64, hh, col0:512], start=False, stop=True),
                       r=[kseg, qrT[cp]], w=[pss], inc=(slot == 1))
                for slot, (hh, kt, i0, col0, pss) in enumerate(info):
                    idx = (i0_, i1_)[slot]
                    if kt >= t0:
                        PE(lambda col0=col0, pss=pss: nc.tensor.matmul(pss[:, col0:col0 + 128], lhsT=ident[:, :], rhs=maskb[:, :], start=False, stop=True),
                           r=[ident, maskb], w=[pss])
                    p_ = pT.get()
                    A(lambda col0=col0, pss=pss, p_=p_: nc.scalar.activation(out=p_[:, col0:512], in_=pss[:, col0:512], func=AF.Exp, scale=ATTN_SCALE),
                      r=[pss], w=[p_])
                    pend[idx] = (p_, i0)

            def emit_PV(idx):
                hh, kt = steps[idx]
                p_, i0 = pend.pop(idx)
                V_ = Vh[(c * 8 + hh) % 2]
                PO = [PS[4], PS[5]]
                for qi in range(i0, QC):
                    po = PO[qi // 2]
                    last = (kt == t0 + qi)
                    PE(lambda qi=qi, po=po, last=last: nc.tensor.matmul(po[:, (qi % 2) * 256:(qi % 2) * 256 + 129], lhsT=p_[:, qi * 128:(qi + 1) * 128],
                                                                        rhs=V_[:, kt, 0:129], start=(kt == 0 and qi % 2 == 0), stop=last),
                       r=[p_, V_], w=[po], inc=(qi == QC - 1))
                if kt == nk - 1 and hh + 2 < 8:
                    kv_load(c, hh + 2)
                if kt == nk - 1:
                    rc_ = rec.get()
                    for b2 in range(2):
                        pov = PO[b2][:, :].rearrange("p (i n) -> p i n", i=2)
                        V(lambda b2=b2, pov=pov: nc.vector.reciprocal(out=rc_[:, 2 * b2:2 * b2 + 2].unsqueeze(2), in_=pov[:, :, 128:129]), r=[PO[b2]], w=[rc_])
                        V(lambda b2=b2, pov=pov: nc.vector.tensor_tensor(out=osb[cp][:, 2 * b2:2 * b2 + 2, hh * 128:(hh + 1) * 128], in0=pov[:, :, 0:128],
                                                                         in1=rc_[:, 2 * b2:2 * b2 + 2].unsqueeze(2).broadcast_to([128, 2, 128]), op=ALU.mult),
                          r=[PO[b2], rc_], w=[osb[cp]])

            npair = len(steps) // 2
            emit_S_pair(0, 1)
            emit_S_pair(2, 3)
            for i in range(npair):
                emit_PV(2 * i)
                yield
                emit_PV(2 * i + 1)
                if i + 2 < npair:
                    emit_S_pair(2 * i + 4, 2 * i + 5)
                yield

        def epilogue(c):
            cp = c % 2
            t0 = 4 * c + 1
            for j in range(QC):
                t = t0 + j
                h = hqe.get()
                ld("sp", h, h[:, :], h_d[t * 128:(t + 1) * 128, :], r=[b_h[t]])
                pv = PS[6][:, :].bitcast(BF16).rearrange("p (c k) -> p c k", c=8)
                for cc in range(8):
                    PE(lambda cc=cc, j=j: nc.tensor.transpose(out=pv[:, cc, :], in_=osb[cp][:, j, cc * 128:(cc + 1) * 128], identity=ident[:, :]),
                       r=[osb[cp], ident], w=[PS[6]], inc=(cc == 7))
                A(lambda: nc.scalar.activation(out=oT[:, :, :], in_=pv, func=AF.Copy), r=[PS[6]], w=[oT])
                yield
                for hf in range(2):
                    yield from idle(2)
                    for cc in range(8):
                        PE(lambda cc=cc, hf=hf: nc.tensor.matmul(PS[7][:, :], lhsT=oT[:, cc, :], rhs=wo[:, cc, hf * 512:(hf + 1) * 512], start=(cc == 0), stop=(cc == 7)),
                           r=[oT, wo], w=[PS[7]], inc=(cc == 7))
                    V(lambda hf=hf, h=h: nc.vector.tensor_tensor(out=h[:, hf * 512:(hf + 1) * 512], in0=h[:, hf * 512:(hf + 1) * 512], in1=PS[7][:, :], op=ALU.add),
                      r=[PS[7]], w=[h])
                    yield
                DMA("pool", lambda e, t=t, h=h: e.dma_start(out=h_d[t * 128:(t + 1) * 128, :], in_=h[:, :]), r=[h], w=[b_h[t]])
                yield from route_and_scatter(1, t, h, h[:, :], PS[6], PS[6], PS[7], rb, bgslack=2)

        def drive(main, bgs, n_main, bg_est=200):
            bgs = list(bgs)
            used = 0
            for i, _ in enumerate(main):
                per = max(1, -(-(bg_est - used) // max(1, n_main - i)))
                budget = per
                while budget > 0 and bgs:
                    try:
                        r_ = next(bgs[0])
                        budget -= 1
                        used += 1
                        if r_ == "idle" and per <= 1:
                            break
                    except StopIteration:
                        bgs.pop(0)
            for g in bgs:
                for _ in g:
                    pass

        for _ in qpath(0):
            pass
        nchunks = 1 if stop_after == "p2b_c0" else 8
        for c in range(nchunks):
            bgs = []
            if c > 0:
                bgs.append(epilogue(c - 1))
            if c + 1 < nchunks:
                bgs.append(qpath(c + 1))
            n_main = 8 * (4 * c + 5)
            drive(attention(c), bgs, n_main)
        for _ in epilogue(nchunks - 1):
            pass

    def pass3():
        common_bufs()
        gf = AT("gf", [128, D], F32)
        ld("sp", gf, gf[:, :], I["gains"].ap()[:, G_FINAL, :])
        hA = [AT(f"hA{i}", [128, D], F32) for i in range(3)]
        ykb = [AT(f"ykb{i}", [128, 2, D], F32) for i in range(3)]
        ob = [AT(f"ob{i}", [128, D], F32) for i in range(2)]

        def tile(t):
            h, y, o = hA[t % 3], ykb[t % 3], ob[t % 2]
            ld("sp", h, h[:, :], h_d[t * 128:(t + 1) * 128, :], r=[b_h[t]])
            ld("sp", y, y[:, :, :], yk_d[1][t * 256:(t + 1) * 256, :].rearrange("(p k) d -> p k d", k=2), r=[b_yk[1]])
            yield
            V(lambda: nc.vector.tensor_tensor(out=h[:, :], in0=h[:, :], in1=y[:, 0, :], op=ALU.add), r=[y], w=[h])
            G(lambda: nc.gpsimd.tensor_tensor(out=h[:, :], in0=h[:, :], in1=y[:, 1, :], op=ALU.add), r=[y], w=[h])
            yield
            s = rstd_of(h, h[:, :], D)
            yield
            V(lambda: nc.vector.scalar_tensor_tensor(out=o[:, :], in0=h[:, :], scalar=s[:, 3:4], in1=gf[:, :], op0=ALU.mult, op1=ALU.mult),
              r=[h, s, gf], w=[o])
            DMA("pool", lambda e, t=t, o=o: e.dma_start(out=out_d[(t - 1) * 128:t * 128, :], in_=o[:, :]), r=[o], s=[b_out])

        pipeline([(lambda t=t: tile(t)) for t in range(1, NT)], depth=3, skew=1)

    pass1()
    if stop_after == "p1":
        if "h_scr" in dbg:
            dbg_dram("h_scr", h_d.ap(), [NT * 128, D], F32, b_h)
        return finish(b_h + [b_xs[0]])
    if "counts" in dbg:
        dbg_out("counts0", base, base[:, :], [128, NE])
    phase("e0")
    expert_loop(0)
    if stop_after == "e0":
        if "yk_scr0" in dbg:
            dbg_dram("yk_scr0", yk_d[0].ap(), [NYK, D], F32, [b_yk[0]])
        return finish(b_h + [b_xs[0], b_yk[0]])
    phase("p2a")
    pass2a()
    if stop_after == "p2a":
        dbg_dram("h_scr", h_d.ap(), [NT * 128, D], F32, b_h)
        dbg_dram("kt", kt_d.ap(), [8, 128, NT, 128], BF16, b_kv)
        dbg_dram("kr", kr_d.ap(), [64, NT, 128], BF16, b_kv)
        dbg_dram("vt", vt_d.ap(), [8, 128, NT, 130], BF16, b_kv)
        return finish(b_h + b_kv)
    phase("p2b")
    pass2b()
    if stop_after in ("p2b", "p2b_c0"):
        dbg_dram("h_scr", h_d.ap(), [NT * 128, D], F32, b_h)
        return finish(b_h + [b_xs[1]])
    if "counts" in dbg:
        dbg_out("counts1", base, base[:, :], [128, NE])
    phase("e1")
    expert_loop(1)
    phase("p3")
    pass3()
    return finish([b_out])


_CONSTS = None


def make_in_maps(inp):
    global _CONSTS
    if _CONSTS is None:
        _CONSTS = _consts()
    f = lambda a: np.ascontiguousarray(np.asarray(a, dtype=np.float32))
    rep = lambda v: np.broadcast_to(f(v)[None, :], (128, f(v).shape[0]))
    gains = np.stack([rep(inp["a_norm"][0]), rep(inp["a_scale"][0]), rep(inp["ffn_norm"][0]), rep(inp["ffn_norm"][1]),
                      rep(inp["kv_norm"]), rep(inp["b_norm"][0]), rep(inp["final_norm"])], axis=1)
    router_w = np.concatenate([f(inp["router_g"]), f(inp["router_e"])], axis=2)
    rb = np.concatenate([f(inp["router_g_bias"]), f(inp["router_e_bias"])], axis=1)
    router_b = np.broadcast_to(rb[None], (128, 2, 36))
    shared = {
        "meta_tokens": f(inp["meta_tokens"]), "a_w": f(inp["a_w"][0]), "gains": np.ascontiguousarray(gains),
        "qn_gain": np.ascontiguousarray(rep(inp["b_q_norm"][0])), "kvl_gain": np.ascontiguousarray(rep(inp["kv_lat_norm"])),
        "b_w_dq": f(inp["b_w_dq"][0]), "b_w_uq": f(inp["b_w_uq"][0]), "b_w_o": f(inp["b_w_o"][0]),
        "w_dkv": f(inp["w_dkv"]), "w_uk": f(inp["w_uk"]).reshape(256, 1024), "w_uv": f(inp["w_uv"]).reshape(256, 1024),
        "router_w": np.ascontiguousarray(router_w), "router_b": np.ascontiguousarray(router_b),
        "w_gate": f(inp["w_gate"]), "w_up": f(inp["w_up"]), "w_down": f(inp["w_down"]),
    }
    shared.update(_CONSTS)
    x = f(inp["x"])
    return [dict(shared, x=x[b]) for b in range(8)]


def kernel(**inputs):
    in_maps = make_in_maps(inputs)
    nc = build_program()
    res = run_bass_kernel_spmd(nc, in_maps, core_ids=list(range(8)))
    return np.stack([np.asarray(r["out"], dtype=np.float32) for r in res.results], axis=0)
```
